# Optimizing a Trainium2 kernel written in Bass

```python
import math
import jax, jax.numpy as jnp
from jax import lax
import numpy as np

D_MODEL = 2048
BATCH = 4
SEQ = 4096
DEPTH = 4

N_MIXERS = 2
N_RET_LAYERS = (DEPTH + 1) // 2
N_POOL_LAYERS = DEPTH // 2
EPS = 1e-6

N_RET_HEADS = 8
RET_QK_DIM = D_MODEL
RET_V_DIM = 2 * D_MODEL
RET_HEAD_QK = RET_QK_DIM // N_RET_HEADS
RET_HEAD_V = RET_V_DIM // N_RET_HEADS
RET_GATE_DIM = RET_V_DIM
RET_IN_COLS = 2 * RET_QK_DIM + RET_V_DIM + RET_GATE_DIM
RET_CHUNK = 128
ROPE_BASE = 10000.0

POOL_WIDTH = 2 * D_MODEL
POOL_WINDOWS = (2, 4, 8, 16)
POOL_GROUPS = len(POOL_WINDOWS)
POOL_GROUP_DIM = POOL_WIDTH // POOL_GROUPS

kernel_name = "hybrid_retention_pooling_gated"


def rms_norm(x, g):
    xf = x.astype(jnp.float32)
    y = xf * lax.rsqrt(jnp.mean(xf * xf, axis=-1, keepdims=True) + EPS)
    return (y * g.astype(jnp.float32)).astype(x.dtype)


def rope(x, seq_len):
    d = x.shape[-1]
    half = d // 2
    inv = ROPE_BASE ** (-jnp.arange(half, dtype=jnp.float32) / half)
    ang = jnp.arange(seq_len, dtype=jnp.float32)[:, None] * inv[None, :]
    cos = jnp.cos(ang)[None, :, None, :]
    sin = jnp.sin(ang)[None, :, None, :]
    x1, x2 = x[..., :half], x[..., half:]
    return jnp.concatenate([x1 * cos - x2 * sin, x1 * sin + x2 * cos], axis=-1)


def retention_log_decay():
    h = jnp.arange(N_RET_HEADS, dtype=jnp.float32)
    return jnp.log1p(-jnp.exp2(-5.0 - h))


def chunkwise_retention(q, k, v):
    B, S, H, dk = q.shape
    dv = v.shape[-1]
    C = RET_CHUNK
    NC = S // C
    lg = retention_log_decay()
    idx = jnp.arange(C, dtype=jnp.float32)
    diff = idx[:, None] - idx[None, :]
    mask = jnp.where(diff[None] >= 0,
                     jnp.exp(jnp.maximum(diff, 0.0)[None] * lg[:, None, None]), 0.0)
    q_decay = jnp.exp((idx[None, :] + 1.0) * lg[:, None])
    k_decay = jnp.exp((C - 1.0 - idx[None, :]) * lg[:, None])
    chunk_decay = jnp.exp(C * lg)

    def to_chunks(t):
        return t.reshape(B, NC, C, H, t.shape[-1]).transpose(1, 0, 3, 2, 4)

    qc, kc, vc = to_chunks(q), to_chunks(k), to_chunks(v)

    def body(state, inp):
        qi, ki, vi = inp
        scores = jnp.einsum('bhid,bhjd->bhij', qi, ki) * mask[None]
        inner = jnp.einsum('bhij,bhje->bhie', scores, vi)
        cross = jnp.einsum('bhid,bhde->bhie', qi * q_decay[None, :, :, None], state)
        new_state = (chunk_decay[None, :, None, None] * state
                     + jnp.einsum('bhjd,bhje->bhde', ki * k_decay[None, :, :, None], vi))
        return new_state, inner + cross

    state0 = jnp.zeros((B, H, dk, dv), jnp.float32)
    _, out = lax.scan(body, state0, (qc, kc, vc))
    return out.transpose(1, 0, 3, 2, 4).reshape(B, S, H, dv)


def retention_layer(x, g_norm, w_in, gn, w_out):
    B, S, _ = x.shape
    h = rms_norm(x, g_norm)
    p = h @ w_in
    q, k, v, z = jnp.split(p, [RET_QK_DIM, 2 * RET_QK_DIM, 2 * RET_QK_DIM + RET_V_DIM], axis=-1)
    q = rope(q.astype(jnp.float32).reshape(B, S, N_RET_HEADS, RET_HEAD_QK), S)
    k = rope(k.astype(jnp.float32).reshape(B, S, N_RET_HEADS, RET_HEAD_QK), S) * (RET_HEAD_QK ** -0.5)
    v = v.astype(jnp.float32).reshape(B, S, N_RET_HEADS, RET_HEAD_V)
    o = chunkwise_retention(q, k, v)
    mu = jnp.mean(o, axis=-1, keepdims=True)
    var = jnp.mean(jnp.square(o - mu), axis=-1, keepdims=True)
    o = ((o - mu) * lax.rsqrt(var + EPS)).reshape(B, S, RET_V_DIM) * gn.astype(jnp.float32)
    o = o.astype(x.dtype) * jax.nn.silu(z)
    return o @ w_out


def pooling_layer(x, g_norm, w_in, w_grp, b_grp, scale, w_out):
    B, S, _ = x.shape
    h = rms_norm(x, g_norm)
    p = h @ w_in
    u, z = jnp.split(p, [POOL_WIDTH], axis=-1)
    u = u.reshape(B, S, POOL_GROUPS, POOL_GROUP_DIM)
    uf = u.astype(jnp.float32)
    cs = jnp.concatenate([jnp.zeros((B, 1, POOL_GROUPS, POOL_GROUP_DIM), jnp.float32),
                          jnp.cumsum(uf, axis=1)], axis=1)
    t = jnp.arange(S)
    pooled = []
    for gi, w in enumerate(POOL_WINDOWS):
        lo = jnp.maximum(t + 1 - w, 0)
        cnt = jnp.minimum(t + 1, w).astype(jnp.float32)[None, :, None]
        win_sum = cs[:, 1:, gi] - jnp.take(cs[:, :, gi], lo, axis=1)
        pooled.append(win_sum / cnt)
    pooled = jnp.stack(pooled, axis=2)
    mix = (pooled - uf).astype(x.dtype)
    mix = jnp.einsum('bsgc,gcd->bsgd', mix, w_grp) + b_grp
    mix = mix.reshape(B, S, POOL_WIDTH) * scale
    return (mix * jax.nn.silu(z)) @ w_out


def setup_inputs(seed: int = 0) -> dict:
    key = jax.random.key(seed)
    ks = jax.random.split(key, 13)
    f32 = jnp.float32
    nr, npl = N_RET_LAYERS, N_POOL_LAYERS
    return {
        "x": jax.random.normal(ks[0], (BATCH, SEQ, D_MODEL), f32),
        "ret_norm": 1.0 + 0.05 * jax.random.normal(ks[1], (nr, D_MODEL), f32),
        "ret_w_in": jax.random.normal(ks[2], (nr, D_MODEL, RET_IN_COLS), f32) * D_MODEL ** -0.5,
        "ret_gn": 1.0 + 0.05 * jax.random.normal(ks[3], (nr, RET_V_DIM), f32),
        "ret_w_out": jax.random.normal(ks[4], (nr, RET_V_DIM, D_MODEL), f32) * RET_V_DIM ** -0.5,
        "pool_norm": 1.0 + 0.05 * jax.random.normal(ks[5], (npl, D_MODEL), f32),
        "pool_w_in": jax.random.normal(ks[6], (npl, D_MODEL, 2 * POOL_WIDTH), f32) * D_MODEL ** -0.5,
        "pool_w_grp": jax.random.normal(ks[7], (npl, POOL_GROUPS, POOL_GROUP_DIM, POOL_GROUP_DIM), f32) * POOL_GROUP_DIM ** -0.5,
        "pool_b_grp": 0.01 * jax.random.normal(ks[8], (npl, POOL_GROUPS, POOL_GROUP_DIM), f32),
        "pool_scale": 1.0 + 0.1 * jax.random.normal(ks[9], (npl, POOL_WIDTH), f32),
        "pool_w_out": jax.random.normal(ks[10], (npl, POOL_WIDTH, D_MODEL), f32) * POOL_WIDTH ** -0.5,
        "final_norm": 1.0 + 0.05 * jax.random.normal(ks[11], (D_MODEL,), f32),
    }


def reference(x, ret_norm, ret_w_in, ret_gn, ret_w_out, pool_norm, pool_w_in,
              pool_w_grp, pool_b_grp, pool_scale, pool_w_out, final_norm):
    h = x
    for i in range(DEPTH):
        j = i // N_MIXERS
        if i % N_MIXERS == 0:
            h = h + retention_layer(h, ret_norm[j], ret_w_in[j], ret_gn[j], ret_w_out[j])
        else:
            h = h + pooling_layer(h, pool_norm[j], pool_w_in[j], pool_w_grp[j],
                                  pool_b_grp[j], pool_scale[j], pool_w_out[j])
    return rms_norm(h, final_norm)
```

```python
import contextlib
import numpy as np
import concourse.bass as bass
import concourse.mybir as mybir
from concourse.bass_utils import run_bass_kernel_spmd

F32 = mybir.dt.float32
BF16 = mybir.dt.bfloat16
AF = mybir.ActivationFunctionType
ALU = mybir.AluOpType

D = 2048
KC = 16
TT = 512
NH = 8
EPS = 1e-6
SEQ = 4096
BATCH = 4
NSLOT = 3
WIN = (2, 4, 8, 16)

ENGS = ("pe", "dve", "act", "pool", "sp")


class Prog:
    def __init__(self, nc):
        self.nc = nc
        self.streams = {e: [] for e in ENGS}
        self.count = {}
        self.known = {e: {} for e in ENGS}
        self.lastw = {}
        self.readers = {}
        self.dma_keys = set()

    def _deps(self, eng, reads, writes):
        deps = {}

        def need(kv, same_ok):
            if kv is None:
                return
            k, v = kv
            if k == eng and same_ok:
                return
            if deps.get(k, 0) < v:
                deps[k] = v

        for r in reads:
            need(self.lastw.get(r), same_ok=(eng == "pe"))
        for w in writes:
            need(self.lastw.get(w), same_ok=True)
            for k, v in self.readers.get(w, {}).items():
                need((k, v), same_ok=True)
        kn = self.known[eng]
        out = []
        for k, v in deps.items():
            if kn.get(k, 0) < v:
                kn[k] = v
                out.append((k, v))
        return out

    def _commit(self, key, val, reads, writes):
        for r in reads:
            self.readers.setdefault(r, {})[key] = val
        for w in writes:
            self.lastw[w] = (key, val)
            self.readers[w] = {}

    def op(self, eng, fn, reads=(), writes=(), inc=True):
        waits = self._deps(eng, reads, writes)
        cur = self.count.get(eng, 0)
        if inc:
            self.count[eng] = cur + 1
        self.streams[eng].append((waits, fn, eng, 1 if inc else 0))
        self._commit(eng, cur + 1, reads, writes)

    def dma(self, q, dsem, out, in_, reads=(), writes=()):
        key = "d:" + dsem
        self.dma_keys.add(key)
        waits = self._deps(q, reads, writes)
        self.count[key] = self.count.get(key, 0) + 16
        self.streams[q].append(
            (waits, lambda e: e.dma_start(out=out, in_=in_), key, 16))
        self._commit(key, self.count[key], reads, writes)

    def fence(self):
        for eng in ENGS:
            waits = []
            for k, v in self.count.items():
                if k == eng:
                    continue
                if self.known[eng].get(k, 0) < v:
                    self.known[eng][k] = v
                    waits.append((k, v))
            self.streams[eng].append((waits, None, None, 0))

    def wait_all(self, eng, res_list):
        waits = self._deps(eng, res_list, ())
        self.streams[eng].append((waits, None, None, 0))

    def emit(self):
        nc = self.nc
        keys = [e for e in ENGS if e != "sp"] + sorted(self.dma_keys)
        with contextlib.ExitStack() as st:
            sems = {k: st.enter_context(nc.semaphore("s_" + k.replace(":", "_")))
                    for k in keys}
            block = st.enter_context(nc.Block())

            def run(e, stream):
                for waits, fn, key, inc in stream:
                    for k, v in waits:
                        e.wait_ge(sems[k], v)
                    if fn is not None:
                        ins = fn(e)
                        if inc:
                            ins.then_inc(sems[key], inc)

            @block.tensor
            def _(e):
                run(e, self.streams["pe"])

            @block.vector
            def _(e):
                run(e, self.streams["dve"])

            @block.scalar
            def _(e):
                run(e, self.streams["act"])

            @block.gpsimd
            def _(e):
                run(e, self.streams["pool"])

            @block.sync
            def _(e):
                run(e, self.streams["sp"])


def const_tables(T):
    half = 128
    inv = 10000.0 ** (-np.arange(half, dtype=np.float64) / half)
    ang = inv[:, None] * np.arange(T, dtype=np.float64)[None, :]
    cosT = np.cos(ang).astype(np.float32)
    sinT = np.sin(ang).astype(np.float32)
    hh = np.arange(NH, dtype=np.float64)
    lg = np.log1p(-np.exp2(-5.0 - hh))
    idx = np.arange(128, dtype=np.float64)
    tri = (idx[None, :] >= idx[:, None]).astype(np.float64)
    maskT = np.exp(-(idx[:, None, None] + 1.0) * lg[None, :, None]) / 16.0 * tri[:, None, :]
    qdec = np.exp((idx[:, None] + 1.0) * lg[None, :])
    kdec = np.exp((127.0 - idx[:, None]) * lg[None, :]) / 16.0
    cdec = [float(np.exp(128.0 * l)) for l in lg]
    ident = np.eye(128, dtype=np.float32)
    ones = np.ones((128, 128), dtype=np.float32)
    acur = np.zeros((128, 4, 128), np.float32)
    aprev = np.zeros((128, 4, 128), np.float32)
    afirst = np.zeros((128, 4, 128), np.float32)
    invc = np.zeros((128, 4, 128), np.float32)
    for g, w in enumerate(WIN):
        for t in range(128):
            for tp in range(t - w + 1, t + 1):
                if tp >= 0:
                    acur[tp, g, t] += 1.0
                    afirst[tp, g, t] += 1.0
                else:
                    aprev[128 + tp, g, t] += 1.0
            acur[t, g, t] -= float(w)
            cnt = min(t + 1, w)
            afirst[t, g, t] -= float(cnt)
            invc[:, g, t] = 1.0 / cnt
    return dict(cosT=cosT, sinT=sinT, maskT=maskT.astype(np.float32).reshape(128, NH * 128),
                qdec=qdec.astype(np.float32), kdec=kdec.astype(np.float32),
                ident=ident, ones=ones,
                acur=acur.reshape(128, 512), aprev=aprev.reshape(128, 512),
                afirst=afirst.reshape(128, 512), invc=invc.reshape(128, 512)), cdec


CONST_SHAPES = lambda T: dict(cosT=[128, T], sinT=[128, T], maskT=[128, NH * 128], qdec=[128, NH],
                              kdec=[128, NH], ident=[128, 128], ones=[128, 128], acur=[128, 512],
                              aprev=[128, 512], afirst=[128, 512], invc=[128, 512])


def const_names(kinds):
    names = {"ones"}
    if "ret" in kinds:
        names |= {"cosT", "sinT", "maskT", "qdec", "kdec", "ident"}
    if "pool" in kinds:
        names |= {"acur", "aprev", "afirst", "invc"}
    return names


def vec_layout(v):
    v = np.asarray(v, np.float32).reshape(-1, 128)
    return np.ascontiguousarray(v.T)


class Builder:
    def __init__(self, T, layers):
        self.T = T
        self.NT = T // TT
        self.layers = layers
        nc = bass.Bass("TRN2", target_bir_lowering=False)
        self.nc = nc
        self.P = Prog(nc)
        self.st = contextlib.ExitStack()
        self.tasks = []
        self.uoff = {}
        self.blkctr = {}
        self.UBYTES = 76 * 1024
        self.out_res = set()
        self.nwt = 0
        self.ctr = {}
        _, self.cdec = const_tables(128)

    def rot(self, name, n):
        c = self.ctr.get(name, 0)
        self.ctr[name] = c + 1
        return c % n

    def sb(self, name, shape, dt):
        return self.st.enter_context(self.nc.sbuf_tensor("sb_" + name, shape, dt))

    def carve(self, setname, shape, dt):
        nbytes = int(np.prod(shape[1:])) * (4 if dt == F32 else 2)
        nbytes = (nbytes + 63) // 64 * 64
        off = self.uoff.get(setname, 0)
        self.uoff[setname] = off + nbytes
        assert off + nbytes <= self.UBYTES, (setname, off + nbytes)
        v = self.U[:, off // 2:(off + nbytes) // 2]
        if dt == F32:
            v = v.bitcast(F32)
        n = int(np.prod(shape[1:]))
        v = v[:, 0:n]
        if len(shape) == 3:
            v = v.rearrange("p (a b) -> p a b", a=shape[1])
        elif len(shape) == 4:
            v = v.rearrange("p (a b c) -> p a b c", a=shape[1], b=shape[2])
        return v

    def bank(self):
        i = self.rot("bank", 6)
        return self.psA[i], "psA%d" % i

    def bankT(self):
        i = self.rot("bankT", 2)
        return self.psT[i], "psT%d" % i

    def mm(self, out, ores, lhsT, rhs, reads, start, stop, inc=None):
        if inc is None:
            inc = stop
        self.P.op("pe", lambda e: e.matmul(out, lhsT, rhs, start=start, stop=stop),
                  reads=reads, writes=[ores], inc=inc)

    def tr(self, out, ores, in_, reads, inc):
        ident = self.ident
        self.P.op("pe", lambda e: e.transpose(out, in_, ident[:, :]),
                  reads=list(reads) + ["const"], writes=[ores], inc=inc)

    def add_task(self, load, compute, key=None):
        if load is not None:
            slot = self.nwt % NSLOT
            self.nwt += 1
            if key is not None:
                li, t = key
                blk = self.blkctr.get(key, 0)
                self.blkctr[key] = blk + 1
                wsc = self.wsc[li]
                res = "wsc%d_%d" % (li, blk)
                load_fp32 = load
                if t == 0:
                    def load(slot, load_fp32=load_fp32, wsc=wsc, blk=blk, res=res):
                        load_fp32(slot)
                        self.P.dma("sp", "wb%d" % slot, wsc[blk], self.W[slot][:, :],
                                   reads=["W%d" % slot], writes=[res])
                else:
                    def load(slot, wsc=wsc, blk=blk, res=res):
                        self.P.dma("pool", "w%d" % slot, self.W[slot][:, :], wsc[blk],
                                   reads=[res], writes=["W%d" % slot])
        else:
            slot = None
        self.tasks.append((load, compute, slot))

    def wload(self, slot, dst_view, src):
        self.P.dma("pool", "w%d" % slot, dst_view, src, writes=["W%d" % slot])

    def build(self):
        nc, T = self.nc, self.T
        dram_in = lambda name, shape: nc.dram_tensor(name, shape, F32, kind="ExternalInput").ap()
        self.hin = dram_in("hin", [D, T])
        self.hout = nc.dram_tensor("hout", [D, T], F32, kind="ExternalOutput").ap()
        self.hbuf = nc.dram_tensor("hbuf", [D, T], F32).ap() if len(self.layers) > 1 else None
        self.cd = {k: dram_in(k, s) for k, s in CONST_SHAPES(T).items() if k in const_names(self.layers)}
        self.lw = []
        for li, kind in enumerate(self.layers):
            p = {}
            if kind == "ret":
                p["norm"] = dram_in("l%d_norm" % li, [128, 16])
                p["w_in"] = dram_in("l%d_w_in" % li, [D, 12288])
                p["gn"] = dram_in("l%d_gn" % li, [128, 32])
                p["w_out"] = dram_in("l%d_w_out" % li, [4096, D])
            elif kind == "pool":
                p["norm"] = dram_in("l%d_norm" % li, [128, 16])
                p["w_in"] = dram_in("l%d_w_in" % li, [D, 8192])
                p["w_grp"] = dram_in("l%d_w_grp" % li, [4, 1024, 1024])
                p["b"] = dram_in("l%d_b" % li, [128, 32])
                p["s"] = dram_in("l%d_s" % li, [128, 32])
                p["w_out"] = dram_in("l%d_w_out" % li, [4096, D])
            else:
                p["norm"] = dram_in("l%d_norm" % li, [128, 16])
            self.lw.append(p)

        self.wsc = {}
        for li, kind in enumerate(self.layers):
            if kind in ("ret", "pool") and self.NT > 1:
                nblk = 32 if kind == "ret" else 28
                self.wsc[li] = nc.dram_tensor("wsc%d" % li, [nblk, 128, 8192], BF16).ap()
        sb = self.sb
        has_ret = "ret" in self.layers
        has_pool = "pool" in self.layers
        self.W = [sb("W%d" % i, [128, 8192], BF16) for i in range(NSLOT)]
        self.hs = [sb("hs%d" % i, [128, TT], F32) for i in range(4)]
        self.hnew = [sb("hnew%d" % i, [128, TT], F32) for i in range(2)]
        self.hn = sb("hn", [128, KC, TT], BF16)
        self.sq = [sb("sq%d" % i, [128, TT], BF16) for i in range(2)]
        self.rt = sb("rt", [128, TT], F32)
        self.rstd = sb("rstd", [128, TT], F32)
        self.gT = sb("gT", [128, 32, TT], BF16)
        self.ident = sb("ident", [128, 128], BF16)
        self.ones = sb("ones", [128, 128], BF16)
        self.epsb = sb("epsb", [128, 1], F32)
        self.vec = [sb("vec%d" % li, [128, 112], F32) for li in range(len(self.layers))]
        self.U = sb("U", [128, self.UBYTES // 2], BF16)
        if has_ret:
            cv = lambda shape, dt: self.carve("ret", shape, dt)
            self.mask = sb("mask", [128, NH, 128], F32)
            self.qdec = sb("qdec", [128, NH], F32)
            self.kdec = sb("kdec", [128, NH], F32)
            self.cos = cv([128, TT], F32)
            self.sin = cv([128, TT], F32)
            self.S = cv([128, NH, 2, 512], F32)
            self.Sbf = [cv([128, 2, 512], BF16) for i in range(2)]
            self.qT = [cv([128, 2, TT], BF16)]
            self.kT = [cv([128, 2, TT], BF16)]
            self.v = [cv([128, 4, 512], BF16)]
            self.kd = [cv([128, 256], BF16) for i in range(2)]
            self.sm = [cv([128, 128], BF16) for i in range(2)]
            self.oraw4 = cv([128, 4, 512], F32)
            self.mv4 = cv([128, 4, 2], F32)
            self.sd4 = cv([128, 4], F32)
            self.rs4 = cv([128, 4], F32)
            self.on = [cv([128, 4, 512], BF16)]
            self.sz = [cv([128, TT], F32) for i in range(2)]
            self.rtmp = [cv([128, TT], F32) for i in range(4)]
            self.st6 = [cv([128, 6], F32) for i in range(4)]
        if has_pool:
            cv = lambda shape, dt: self.carve("pool", shape, dt)
            self.acur = sb("acur", [128, 4, 128], BF16)
            self.aprev = sb("aprev", [128, 4, 128], BF16)
            self.afirst = sb("afirst", [128, 4, 128], BF16)
            self.invc = sb("invc", [128, 4, 128], F32)
            self.bs = [sb("bs%d" % li, [128, 32], F32) for li in range(len(self.layers))]
            self.u = cv([128, 4, 4096], BF16)
            self.uprev = cv([128, 4096], BF16)
            self.mixs = [cv([128, 8, TT], BF16) for i in range(2)]
            self.m2 = [cv([128, TT], F32) for i in range(2)]
        self.psA = [self.st.enter_context(nc.psum_tensor("psA%d" % i, [128, 512], F32)) for i in range(6)]
        self.psT = [self.st.enter_context(nc.psum_tensor("psT%d" % i, [128, 1024], BF16)) for i in range(2)]

        P = self.P
        if has_ret:
            P.dma("pool", "c0", self.ident[:, :], self.cd["ident"][:, :], writes=["const"])
        P.dma("pool", "c1", self.ones[:, :], self.cd["ones"][:, :], writes=["const1"])
        epsb = self.epsb
        P.op("dve", lambda e: e.memset(epsb[:, :], EPS), writes=["epsb"])
        if has_ret:
            P.dma("sp", "c2", self.mask[:, :, :], self.cd["maskT"].rearrange("p (h i) -> p h i", h=NH), writes=["mask"])
            P.dma("sp", "c3", self.qdec[:, :], self.cd["qdec"][:, :], writes=["qdec"])
            P.dma("sp", "c4", self.kdec[:, :], self.cd["kdec"][:, :], writes=["kdec"])
        if has_pool:
            for nm, key in (("acur", "c5"), ("aprev", "c6"), ("afirst", "c7")):
                P.dma("pool", key, getattr(self, nm)[:, :, :],
                      self.cd[nm].rearrange("p (g t) -> p g t", g=4), writes=[nm])
            P.dma("sp", "c8", self.invc[:, :, :], self.cd["invc"].rearrange("p (g t) -> p g t", g=4), writes=["invc"])
        for li, kind in enumerate(self.layers):
            p = self.lw[li]
            vec = self.vec[li]
            P.dma("sp", "v%d" % li, vec[:, 0:16], p["norm"][:, :], writes=["vec%d" % li])
            if kind == "ret":
                P.dma("sp", "v%d" % li, vec[:, 16:48], p["gn"][:, :], writes=["vec%d" % li])
            elif kind == "pool":
                P.dma("sp", "v%d" % li, vec[:, 16:48], p["b"][:, :], writes=["vec%d" % li])
                P.dma("sp", "v%d" % li, vec[:, 48:80], p["s"][:, :], writes=["vec%d" % li])
                bs = self.bs[li]
                self._tt("dve", bs[:, :], vec[:, 16:48], vec[:, 48:80], ALU.mult,
                         ["vec%d" % li], ["bs%d" % li])

        nl = len(self.layers)
        for li, kind in enumerate(self.layers):
            src = self.hin if li == 0 else self.hbuf
            dst = self.hout if li == nl - 1 else self.hbuf
            if li > 0:
                self.add_task(None, lambda slot: self.P.fence())
            if kind == "ret":
                self.ret_layer(li, src, dst)
            elif kind == "pool":
                self.pool_layer(li, src, dst)
            else:
                self.fnorm_layer(li, src, dst)

        wt = [i for i, t in enumerate(self.tasks) if t[0] is not None]
        nl_issued = 0
        wt_done = 0
        for i, (load, compute, slot) in enumerate(self.tasks):
            while nl_issued < len(wt) and nl_issued < wt_done + NSLOT:
                j = wt[nl_issued]
                self.tasks[j][0](self.tasks[j][2])
                nl_issued += 1
            compute(slot)
            if load is not None:
                wt_done += 1
        P.wait_all("sp", sorted(self.out_res))
        P.emit()
        self.st.close()
        return nc

    def _tt(self, eng, out, in0, in1, op, reads, writes):
        self.P.op(eng, lambda e: e.tensor_tensor(out, in0, in1, op), reads=reads, writes=writes)

    def _ts(self, eng, out, in0, s1, s2, op0, op1, reads, writes):
        if s2 is None:
            self.P.op(eng, lambda e: e.tensor_scalar(out, in0, s1, None, op0), reads=reads, writes=writes)
        else:
            self.P.op(eng, lambda e: e.tensor_scalar(out, in0, s1, s2, op0, op1), reads=reads, writes=writes)

    def _stt(self, eng, out, in0, scalar, in1, op0, op1, reads, writes):
        self.P.op(eng, lambda e: e.scalar_tensor_tensor(out, in0, scalar, in1, op0, op1),
                  reads=reads, writes=writes)

    def _act(self, out, in_, func, reads, writes, bias=None, scale=None):
        kw = {}
        if bias is not None:
            kw["bias"] = bias
        if scale is not None:
            kw["scale"] = scale
        self.P.op("act", lambda e: e.activation(out, in_, func, **kw), reads=reads, writes=writes)

    def _copy(self, eng, out, in_, reads, writes):
        self.P.op(eng, lambda e: e.tensor_copy(out, in_), reads=reads, writes=writes)

    def norm_stage(self, li, src, t, final_dst=None):
        P = self.P
        vec = self.vec[li]
        cols = slice(t * TT, (t + 1) * TT)
        srcv = src.rearrange("(k p) t -> p k t", p=128)
        ps, pres = self.bank()
        for kc in range(KC):
            s = self.rot("hs", 4)
            P.dma("sp", "hs%d" % s, self.hs[s][:, :], srcv[:, kc, cols], reads=["h%d_%d" % (t, kc)], writes=["hs%d" % s])
            q = self.rot("sq", 2)
            self._act(self.sq[q][:, :], self.hs[s][:, :], AF.Square, ["hs%d" % s], ["sq%d" % q])
            self.mm(ps[:, :], pres, self.ones[:, :], self.sq[q][:, :], ["const1", "sq%d" % q],
                    kc == 0, kc == KC - 1, inc=True)
        self._ts("dve", self.rt[:, :], ps[:, :], 1.0 / D, EPS, ALU.mult, ALU.add, [pres], ["rt"])
        self._act(self.rt[:, :], self.rt[:, :], AF.Sqrt, ["rt"], ["rt"])
        rstd, rt = self.rstd, self.rt
        P.op("dve", lambda e: e.reciprocal(rstd[:, :], rt[:, :]), reads=["rt"], writes=["rstd"])
        for kc in range(KC):
            s = self.rot("hs", 4)
            P.dma("sp", "hs%d" % s, self.hs[s][:, :], srcv[:, kc, cols], reads=["h%d_%d" % (t, kc)], writes=["hs%d" % s])
            if final_dst is None:
                self._stt("dve", self.hn[:, kc, :], self.hs[s][:, :], vec[:, kc:kc + 1], self.rstd[:, :],
                          ALU.mult, ALU.mult, ["hs%d" % s, "rstd", "vec%d" % li], ["hn%d" % kc])
            else:
                o = self.rot("hnew", 2)
                self._stt("dve", self.hnew[o][:, :], self.hs[s][:, :], vec[:, kc:kc + 1], self.rstd[:, :],
                          ALU.mult, ALU.mult, ["hs%d" % s, "rstd", "vec%d" % li], ["hnew%d" % o])
                dstv = final_dst.rearrange("(k p) t -> p k t", p=128)
                P.dma("sp", "ho%d" % o, dstv[:, kc, cols], self.hnew[o][:, :],
                      reads=["hnew%d" % o], writes=["f%d_%d" % (t, kc)])
                self.out_res.add("f%d_%d" % (t, kc))

    def fnorm_layer(self, li, src, dst):
        for t in range(self.NT):
            self.add_task(None, lambda slot, t=t: self.norm_stage(li, src, t, final_dst=dst))

    def out_tasks(self, li, src, dst, t):
        P = self.P
        w_out = self.lw[li]["w_out"].rearrange("(k p) c -> p k c", p=128)
        cols = slice(t * TT, (t + 1) * TT)
        srcv = src.rearrange("(k p) t -> p k t", p=128)
        dstv = dst.rearrange("(k p) t -> p k t", p=128)
        for j in range(8):
            def load(slot, j=j):
                Wv = self.W[slot][:, :].rearrange("p (k c) -> p k c", k=32)
                self.wload(slot, Wv, w_out[:, :, j * 256:(j + 1) * 256])

            def compute(slot, j=j):
                Wv = self.W[slot][:, :].rearrange("p (k c) -> p k c", k=32)
                for dl in range(2):
                    dc = j * 2 + dl
                    ps, pres = self.bank()
                    for kc in range(32):
                        self.mm(ps[:, :], pres, Wv[:, kc, dl * 128:(dl + 1) * 128], self.gT[:, kc, :],
                                ["W%d" % slot, "gT%d" % kc], kc == 0, kc == 31)
                    s = self.rot("hs", 4)
                    hres = "h%d_%d" % (t, dc)
                    P.dma("sp", "hs%d" % s, self.hs[s][:, :], srcv[:, dc, cols], reads=[hres], writes=["hs%d" % s])
                    o = self.rot("hnew", 2)
                    self._tt("dve", self.hnew[o][:, :], ps[:, :], self.hs[s][:, :], ALU.add,
                             [pres, "hs%d" % s], ["hnew%d" % o])
                    P.dma("sp", "ho%d" % o, dstv[:, dc, cols], self.hnew[o][:, :],
                          reads=["hnew%d" % o], writes=[hres])
                    self.out_res.add(hres)
            self.add_task(load, compute, key=(li, t) if self.NT > 1 else None)

    def pool_layer(self, li, src, dst):
        P = self.P
        p = self.lw[li]
        vec = self.vec[li]
        w_in = p["w_in"].rearrange("(k p) c -> p k c", p=128)
        w_grp = p["w_grp"].rearrange("g (k p) c -> g p k c", p=128)
        uprev = self.uprev
        self.add_task(None, lambda slot: P.op("pool", lambda e: e.memset(uprev[:, :], 0.0), writes=["uprev"]))
        for t in range(self.NT):
            self.add_task(None, lambda slot, t=t: self.norm_stage(li, src, t))
            for j in range(8):
                def load(slot, j=j):
                    Wv = self.W[slot][:, :].rearrange("p (k c) -> p k c", k=16)
                    self.wload(slot, Wv, w_in[:, :, j * 512:(j + 1) * 512])

                def compute(slot, j=j):
                    Wv = self.W[slot][:, :].rearrange("p (k c) -> p k c", k=16)
                    for tc in range(4):
                        ps, pres = self.bank()
                        for kc in range(KC):
                            self.mm(ps[:, :], pres, self.hn[:, kc, tc * 128:(tc + 1) * 128], Wv[:, kc, :],
                                    ["hn%d" % kc, "W%d" % slot], kc == 0, kc == KC - 1)
                        dstu = self.u[:, tc, j * 512:(j + 1) * 512]
                        if (tc + j) % 2 == 0:
                            self._act(dstu, ps[:, :], AF.Copy, [pres], ["u%d_%d" % (tc, j)])
                        else:
                            self._copy("dve", dstu, ps[:, :], [pres], ["u%d_%d" % (tc, j)])
                self.add_task(load, compute, key=(li, t) if self.NT > 1 else None)
            for j in range(8):
                def load(slot, j=j):
                    Wv = self.W[slot][:, :].rearrange("p (k c) -> p k c", k=16)
                    self.wload(slot, Wv, w_in[:, :, 4096 + j * 512:4096 + (j + 1) * 512])

                def compute(slot, j=j):
                    Wv = self.W[slot][:, :].rearrange("p (k c) -> p k c", k=16)
                    for q in range(4):
                        oc = j * 4 + q
                        ps, pres = self.bank()
                        for kc in range(KC):
                            self.mm(ps[:, :], pres, Wv[:, kc, q * 128:(q + 1) * 128], self.hn[:, kc, :],
                                    ["W%d" % slot, "hn%d" % kc], kc == 0, kc == KC - 1)
                        self._act(self.gT[:, oc, :], ps[:, :], AF.Silu, [pres], ["gT%d" % oc])
                self.add_task(load, compute, key=(li, t) if self.NT > 1 else None)
            for g in range(4):
                def load(slot, g=g):
                    Wv = self.W[slot][:, :].rearrange("p (k c) -> p k c", k=8)
                    self.wload(slot, Wv, w_grp[g])

                def compute(slot, g=g, t=t):
                    Wv = self.W[slot][:, :].rearrange("p (k c) -> p k c", k=8)
                    mx = self.mixs[g % 2]
                    mres = "mixs%d" % (g % 2)
                    j0 = g * 2
                    for cl in range(8):
                        cc = g * 8 + cl
                        ublk = cc // 4
                        ps, pres = self.bank()
                        for tc in range(4):
                            first = (t == 0 and tc == 0)
                            A = (self.afirst if first else self.acur)
                            ares = "afirst" if first else "acur"
                            cur = self.u[:, tc, cc * 128:(cc + 1) * 128]
                            if tc == 0:
                                prv = self.uprev[:, cc * 128:(cc + 1) * 128]
                                prv_res = "uprev"
                            else:
                                prv = self.u[:, tc - 1, cc * 128:(cc + 1) * 128]
                                prv_res = "u%d_%d" % (tc - 1, ublk)
                            o = ps[:, tc * 128:(tc + 1) * 128]
                            self.mm(o, pres, cur, A[:, g, :], ["u%d_%d" % (tc, ublk), ares], True, False, inc=False)
                            self.mm(o, pres, prv, self.aprev[:, g, :], [prv_res, "aprev"], False, True,
                                    inc=(tc == 3))
                        if t == 0:
                            self._tt("dve", mx[:, cl, 0:128], ps[:, 0:128], self.invc[:, g, :], ALU.mult,
                                     [pres, "invc"], [mres + "_%d" % cl])
                            self._ts("dve", mx[:, cl, 128:TT], ps[:, 128:TT], 1.0 / WIN[g], None, ALU.mult, None,
                                     [pres], [mres + "_%db" % cl])
                        else:
                            self._ts("dve", mx[:, cl, :], ps[:, :], 1.0 / WIN[g], None, ALU.mult, None,
                                     [pres], [mres + "_%d" % cl, mres + "_%db" % cl])
                    for jd in range(8):
                        oc = g * 8 + jd
                        ps, pres = self.bank()
                        for kc in range(8):
                            self.mm(ps[:, :], pres, Wv[:, kc, jd * 128:(jd + 1) * 128], mx[:, kc, :],
                                    ["W%d" % slot, mres + "_%d" % kc, mres + "_%db" % kc], kc == 0, kc == 7)
                        m = self.rot("m2", 2)
                        self._act(self.m2[m][:, :], ps[:, :], AF.Identity, [pres, "vec%d" % li, "bs%d" % li],
                                  ["m2_%d" % m], bias=self.bs[li][:, oc:oc + 1], scale=vec[:, 48 + oc:49 + oc])
                        eng = "dve" if jd % 2 == 0 else "pool"
                        self._tt(eng, self.gT[:, oc, :], self.m2[m][:, :], self.gT[:, oc, :], ALU.mult,
                                 ["m2_%d" % m, "gT%d" % oc], ["gT%d" % oc])
                    if g == 3:
                        ures = ["u3_%d" % b for b in range(8)]
                        self._copy("pool", self.uprev[:, :], self.u[:, 3, :], ures, ["uprev"])
                self.add_task(load, compute, key=(li, t) if self.NT > 1 else None)
            self.out_tasks(li, src, dst, t)

    def ret_layer(self, li, src, dst):
        P = self.P
        p = self.lw[li]
        vec = self.vec[li]
        w_in = p["w_in"].rearrange("(k p) c -> p k c", p=128)
        S = self.S
        self.add_task(None, lambda slot: P.op("pool", lambda e: e.memset(S[:, :, :, :], 0.0),
                                              writes=["S%d" % h for h in range(NH)]))
        for t in range(self.NT):
            def pre(slot, t=t):
                self.norm_stage(li, src, t)
                cols = slice(t * TT, (t + 1) * TT)
                P.dma("sp", "cos", self.cos[:, :], self.cd["cosT"][:, cols], writes=["cos"])
                P.dma("sp", "sin", self.sin[:, :], self.cd["sinT"][:, cols], writes=["sin"])
            self.add_task(None, pre)
            for h in range(NH):
                hb = 0

                def load_qk(slot, h=h):
                    Wv = self.W[slot][:, :].rearrange("p (k c) -> p k c", k=16)
                    self.wload(slot, Wv[:, :, 0:256], w_in[:, :, h * 256:(h + 1) * 256])
                    self.wload(slot, Wv[:, :, 256:512], w_in[:, :, 2048 + h * 256:2048 + (h + 1) * 256])

                def comp_qk(slot, h=h, hb=hb):
                    Wv = self.W[slot][:, :].rearrange("p (k c) -> p k c", k=16)
                    for wi, (dstT, dres) in enumerate(((self.qT[hb], "qT%d" % hb), (self.kT[hb], "kT%d" % hb))):
                        pa, para = self.bank()
                        pb, parb = self.bank()
                        for half, (ps, pres) in enumerate(((pa, para), (pb, parb))):
                            c0 = wi * 256 + half * 128
                            for kc in range(KC):
                                self.mm(ps[:, :], pres, Wv[:, kc, c0:c0 + 128], self.hn[:, kc, :],
                                        ["W%d" % slot, "hn%d" % kc], kc == 0, kc == KC - 1)
                        r = self.rtmp
                        self._tt("dve", r[0][:, :], pa[:, :], self.cos[:, :], ALU.mult, [para, "cos"], ["rtmp0"])
                        self._tt("dve", r[1][:, :], pb[:, :], self.sin[:, :], ALU.mult, [parb, "sin"], ["rtmp1"])
                        self._tt("pool", dstT[:, 0, :], r[0][:, :], r[1][:, :], ALU.subtract,
                                 ["rtmp0", "rtmp1"], [dres])
                        self._tt("dve", r[2][:, :], pa[:, :], self.sin[:, :], ALU.mult, [para, "sin"], ["rtmp2"])
                        self._tt("dve", r[3][:, :], pb[:, :], self.cos[:, :], ALU.mult, [parb, "cos"], ["rtmp3"])
                        self._tt("pool", dstT[:, 1, :], r[2][:, :], r[3][:, :], ALU.add,
                                 ["rtmp2", "rtmp3"], [dres])
                self.add_task(load_qk, comp_qk, key=(li, t) if self.NT > 1 else None)

                def load_v(slot, h=h):
                    Wv = self.W[slot][:, :].rearrange("p (k c) -> p k c", k=16)
                    self.wload(slot, Wv, w_in[:, :, 4096 + h * 512:4096 + (h + 1) * 512])

                def comp_v(slot, h=h, hb=hb, t=t):
                    Wv = self.W[slot][:, :].rearrange("p (k c) -> p k c", k=16)
                    vb, vres = self.v[hb], "v%d" % hb
                    for tc in range(4):
                        ps, pres = self.bank()
                        for kc in range(KC):
                            self.mm(ps[:, :], pres, self.hn[:, kc, tc * 128:(tc + 1) * 128], Wv[:, kc, :],
                                    ["hn%d" % kc, "W%d" % slot], kc == 0, kc == KC - 1)
                        self._act(vb[:, tc, :], ps[:, :], AF.Copy, [pres], [vres + "_%d" % tc])
                    self.ret_core(li, h, hb)
                self.add_task(load_v, comp_v, key=(li, t) if self.NT > 1 else None)

                def load_z(slot, h=h):
                    Wv = self.W[slot][:, :].rearrange("p (k c) -> p k c", k=16)
                    self.wload(slot, Wv, w_in[:, :, 8192 + h * 512:8192 + (h + 1) * 512])

                def comp_z(slot, h=h, hb=hb):
                    Wv = self.W[slot][:, :].rearrange("p (k c) -> p k c", k=16)
                    onb, onres = self.on[hb], "on%d" % hb
                    for ec in range(4):
                        oc = h * 4 + ec
                        ps, pres = self.bank()
                        for kc in range(KC):
                            self.mm(ps[:, :], pres, Wv[:, kc, ec * 128:(ec + 1) * 128], self.hn[:, kc, :],
                                    ["W%d" % slot, "hn%d" % kc], kc == 0, kc == KC - 1)
                        z = self.rot("sz", 2)
                        self._act(self.sz[z][:, :], ps[:, :], AF.Silu, [pres], ["sz%d" % z])
                        pT, ptres = self.bankT()
                        for tc in range(4):
                            self.tr(pT[:, tc * 128:(tc + 1) * 128], ptres, onb[:, tc, ec * 128:(ec + 1) * 128],
                                    [onres + "_%d" % tc], inc=(tc == 3))
                        self._stt("dve", self.gT[:, oc, :], pT[:, 0:TT], vec[:, 16 + oc:17 + oc], self.sz[z][:, :],
                                  ALU.mult, ALU.mult, [ptres, "vec%d" % li, "sz%d" % z], ["gT%d" % oc])
                self.add_task(load_z, comp_z, key=(li, t) if self.NT > 1 else None)
            self.out_tasks(li, src, dst, t)

    def ret_core(self, li, h, hb):
        P = self.P
        qT, kT, vb, onb = self.qT[hb], self.kT[hb], self.v[hb], self.on[hb]
        qres, kres, vres, onres = "qT%d" % hb, "kT%d" % hb, "v%d" % hb, "on%d" % hb
        S = self.S
        Sres = "S%d" % h
        g128 = self.cdec[h]
        for dch in range(2):
            self._copy("pool", self.Sbf[0][:, dch, :], S[:, h, dch, :], [Sres], ["Sbf0_%d" % dch])
        for tc in range(4):
            par = tc % 2
            tcs = slice(tc * 128, (tc + 1) * 128)
            pT, ptres = self.bankT()
            for dch in range(2):
                self.tr(pT[:, dch * 128:(dch + 1) * 128], ptres, kT[:, dch, tcs], [kres], inc=(dch == 1))
            x = self.rot("kd", 2)
            self._act(self.kd[x][:, :], pT[:, 0:256], AF.Identity, [ptres, "kdec"], ["kd%d" % x],
                      scale=self.kdec[:, h:h + 1])
            pss, psres = self.bank()
            for dch in range(2):
                self.mm(pss[:, 0:128], psres, kT[:, dch, tcs], qT[:, dch, tcs], [kres, qres], dch == 0, dch == 1)
            y = self.rot("sm", 2)
            self._tt("dve", self.sm[y][:, :], pss[:, 0:128], self.mask[:, h, :], ALU.mult,
                     [psres, "mask"], ["sm%d" % y])
            psS = []
            for dch in range(2):
                ps, pres = self.bank()
                self.mm(ps[:, :], pres, self.kd[x][:, dch * 128:(dch + 1) * 128], vb[:, tc, :],
                        ["kd%d" % x, vres + "_%d" % tc], True, True)
                psS.append((ps, pres))
            pso, pores = self.bank()
            self.mm(pso[:, :], pores, self.sm[y][:, :], vb[:, tc, :], ["sm%d" % y, vres + "_%d" % tc], True, False,
                    inc=False)
            for dch in range(2):
                self.mm(pso[:, :], pores, qT[:, dch, tcs], self.Sbf[par][:, dch, :],
                        [qres, "Sbf%d_%d" % (par, dch)], False, dch == 1)
            for dch in range(2):
                ps, pres = psS[dch]
                self._stt("dve", S[:, h, dch, :], S[:, h, dch, :], g128, ps[:, :], ALU.mult, ALU.add,
                          [Sres, pres], [Sres])
                if tc < 3:
                    self._copy("pool", self.Sbf[1 - par][:, dch, :], S[:, h, dch, :], [Sres],
                               ["Sbf%d_%d" % (1 - par, dch)])
            orw = self.oraw4
            self._act(orw[:, tc, :], pso[:, :], AF.Identity, [pores, "qdec"], ["oraw_%d" % tc],
                      scale=self.qdec[:, h:h + 1])
            m = self.rot("stat", 4)
            st6, mv4 = self.st6[m], self.mv4
            P.op("dve", lambda e, st6=st6, tc=tc: e.bn_stats(st6[:, :], orw[:, tc, :]),
                 reads=["oraw_%d" % tc], writes=["st6_%d" % m])
            P.op("dve", lambda e, st6=st6, tc=tc: e.bn_aggr(mv4[:, tc, :], st6[:, :]),
                 reads=["st6_%d" % m], writes=["mv4_%d" % tc])
        sd4, rs4, mv4, orw = self.sd4, self.rs4, self.mv4, self.oraw4
        mvr = ["mv4_%d" % tc for tc in range(4)]
        self._act(sd4[:, :], mv4[:, :, 1], AF.Sqrt, mvr + ["epsb"], ["sd4"], bias=self.epsb[:, 0:1], scale=1.0)
        P.op("dve", lambda e: e.reciprocal(rs4[:, :], sd4[:, :]), reads=["sd4"], writes=["rs4"])
        for tc in range(4):
            self._ts("pool", onb[:, tc, :], orw[:, tc, :], mv4[:, tc, 0:1], rs4[:, tc:tc + 1], ALU.subtract, ALU.mult,
                     ["oraw_%d" % tc, "mv4_%d" % tc, "rs4"], [onres + "_%d" % tc])


_CACHE = {}


def get_prog(T, layers):
    key = (T, tuple(layers))
    if key not in _CACHE:
        _CACHE[key] = Builder(T, list(layers)).build()
    return _CACHE[key]


def layer_inputs(li, kind, j, inp):
    d = {}
    if kind == "ret":
        d["l%d_norm" % li] = vec_layout(inp["ret_norm"][j])
        d["l%d_w_in" % li] = np.ascontiguousarray(inp["ret_w_in"][j], dtype=np.float32)
        d["l%d_gn" % li] = vec_layout(inp["ret_gn"][j])
        d["l%d_w_out" % li] = np.ascontiguousarray(inp["ret_w_out"][j], dtype=np.float32)
    elif kind == "pool":
        d["l%d_norm" % li] = vec_layout(inp["pool_norm"][j])
        d["l%d_w_in" % li] = np.ascontiguousarray(inp["pool_w_in"][j], dtype=np.float32)
        d["l%d_w_grp" % li] = np.ascontiguousarray(inp["pool_w_grp"][j], dtype=np.float32)
        d["l%d_b" % li] = vec_layout(np.asarray(inp["pool_b_grp"][j]).reshape(-1))
        d["l%d_s" % li] = vec_layout(inp["pool_scale"][j])
        d["l%d_w_out" % li] = np.ascontiguousarray(inp["pool_w_out"][j], dtype=np.float32)
    else:
        d["l%d_norm" % li] = vec_layout(inp["final_norm"])
    return d


FUSED = True


def kernel(**inputs):
    inp = {k: np.asarray(v) for k, v in inputs.items()}
    x = inp["x"].astype(np.float32, copy=False)
    B, S, _ = x.shape
    consts, _ = const_tables(S)
    hT = [np.ascontiguousarray(x[b].T) for b in range(B)]
    cores = list(range(B))
    plan = [("ret", 0), ("pool", 0), ("ret", 1), ("pool", 1), ("fnorm", 0)]
    if FUSED:
        stages = [plan]
    else:
        stages = [[s] for s in plan]
    for stage in stages:
        kinds = [k for k, _ in stage]
        nc = get_prog(S, kinds)
        shared = {k: v for k, v in consts.items() if k in const_names(kinds)}
        for li, (kind, j) in enumerate(stage):
            shared.update(layer_inputs(li, kind, j, inp))
        in_maps = []
        for b in range(B):
            m = dict(shared)
            m["hin"] = hT[b]
            in_maps.append(m)
        res = run_bass_kernel_spmd(nc, in_maps, core_ids=cores)
        hT = [np.asarray(res.results[b]["hout"]) for b in range(B)]
    out = np.stack([h.T for h in hT], axis=0).astype(np.float32)
    return out
```

```python
import contextlib
import numpy as np
import concourse.bass as bass
import concourse.mybir as mybir
from concourse.bass_utils import run_bass_kernel_spmd

F32 = mybir.dt.float32
BF16 = mybir.dt.bfloat16
AF = mybir.ActivationFunctionType
ALU = mybir.AluOpType

D = 2048
KC = 16
TT = 512
NH = 8
EPS = 1e-6
SEQ = 4096
BATCH = 4
NSLOT = 3
WIN = (2, 4, 8, 16)

ENGS = ("pe", "dve", "act", "pool", "sp")


class Prog:
    def __init__(self, nc):
        self.nc = nc
        self.streams = {e: [] for e in ENGS}
        self.count = {}
        self.known = {e: {} for e in ENGS}
        self.lastw = {}
        self.readers = {}
        self.dma_keys = set()

    def _deps(self, eng, reads, writes):
        deps = {}

        def need(kv, same_ok):
            if kv is None:
                return
            k, v = kv
            if k == eng and same_ok:
                return
            if deps.get(k, 0) < v:
                deps[k] = v

        for r in reads:
            need(self.lastw.get(r), same_ok=(eng == "pe"))
        for w in writes:
            need(self.lastw.get(w), same_ok=True)
            for k, v in self.readers.get(w, {}).items():
                need((k, v), same_ok=True)
        kn = self.known[eng]
        out = []
        for k, v in deps.items():
            if kn.get(k, 0) < v:
                kn[k] = v
                out.append((k, v))
        return out

    def _commit(self, key, val, reads, writes):
        for r in reads:
            self.readers.setdefault(r, {})[key] = val
        for w in writes:
            self.lastw[w] = (key, val)
            self.readers[w] = {}

    def op(self, eng, fn, reads=(), writes=(), inc=True):
        waits = self._deps(eng, reads, writes)
        cur = self.count.get(eng, 0)
        if inc:
            self.count[eng] = cur + 1
        self.streams[eng].append((waits, fn, eng, 1 if inc else 0))
        self._commit(eng, cur + 1, reads, writes)

    def dma(self, q, dsem, out, in_, reads=(), writes=()):
        key = "d:" + dsem
        self.dma_keys.add(key)
        waits = self._deps(q, reads, writes)
        self.count[key] = self.count.get(key, 0) + 16
        self.streams[q].append(
            (waits, lambda e: e.dma_start(out=out, in_=in_), key, 16))
        self._commit(key, self.count[key], reads, writes)

    def dmaop(self, q, dsem, fn, reads=(), writes=()):
        key = "d:" + dsem
        self.dma_keys.add(key)
        waits = self._deps(q, reads, writes)
        self.count[key] = self.count.get(key, 0) + 16
        self.streams[q].append((waits, fn, key, 16))
        self._commit(key, self.count[key], reads, writes)

    def fence(self):
        for eng in ENGS:
            waits = []
            for k, v in self.count.items():
                if k == eng:
                    continue
                if self.known[eng].get(k, 0) < v:
                    self.known[eng][k] = v
                    waits.append((k, v))
            self.streams[eng].append((waits, None, None, 0))

    def wait_all(self, eng, res_list):
        waits = self._deps(eng, res_list, ())
        self.streams[eng].append((waits, None, None, 0))

    def emit(self):
        nc = self.nc
        keys = [e for e in ENGS if e != "sp"] + sorted(self.dma_keys)
        with contextlib.ExitStack() as st:
            sems = {k: st.enter_context(nc.semaphore("s_" + k.replace(":", "_")))
                    for k in keys}
            block = st.enter_context(nc.Block())

            def run(e, stream):
                for waits, fn, key, inc in stream:
                    for k, v in waits:
                        e.wait_ge(sems[k], v)
                    if fn is not None:
                        ins = fn(e)
                        if inc:
                            ins.then_inc(sems[key], inc)

            @block.tensor
            def _(e):
                run(e, self.streams["pe"])

            @block.vector
            def _(e):
                run(e, self.streams["dve"])

            @block.scalar
            def _(e):
                run(e, self.streams["act"])

            @block.gpsimd
            def _(e):
                run(e, self.streams["pool"])

            @block.sync
            def _(e):
                run(e, self.streams["sp"])


def const_tables(T):
    half = 128
    inv = 10000.0 ** (-np.arange(half, dtype=np.float64) / half)
    ang = inv[:, None] * np.arange(T, dtype=np.float64)[None, :]
    cosT = np.cos(ang).astype(np.float32)
    sinT = np.sin(ang).astype(np.float32)
    hh = np.arange(NH, dtype=np.float64)
    lg = np.log1p(-np.exp2(-5.0 - hh))
    idx = np.arange(128, dtype=np.float64)
    tri = (idx[None, :] >= idx[:, None]).astype(np.float64)
    maskT = np.exp(-(idx[:, None, None] + 1.0) * lg[None, :, None]) / 16.0 * tri[:, None, :]
    qdec = np.exp((idx[:, None] + 1.0) * lg[None, :])
    kdec = np.exp((127.0 - idx[:, None]) * lg[None, :]) / 16.0
    cdec = [float(np.exp(128.0 * l)) for l in lg]
    ident = np.eye(128, dtype=np.float32)
    ones = np.ones((128, 128), dtype=np.float32)
    acur = np.zeros((128, 4, 128), np.float32)
    aprev = np.zeros((128, 4, 128), np.float32)
    afirst = np.zeros((128, 4, 128), np.float32)
    invc = np.zeros((128, 4, 128), np.float32)
    for g, w in enumerate(WIN):
        for t in range(128):
            for tp in range(t - w + 1, t + 1):
                if tp >= 0:
                    acur[tp, g, t] += 1.0
                    afirst[tp, g, t] += 1.0
                else:
                    aprev[128 + tp, g, t] += 1.0
            acur[t, g, t] -= float(w)
            cnt = min(t + 1, w)
            afirst[t, g, t] -= float(cnt)
            invc[:, g, t] = 1.0 / cnt
    return dict(cosT=cosT, sinT=sinT, maskT=maskT.astype(np.float32).reshape(128, NH * 128),
                qdec=qdec.astype(np.float32), kdec=kdec.astype(np.float32),
                ident=ident, ones=ones,
                acur=acur.reshape(128, 512), aprev=aprev.reshape(128, 512),
                afirst=afirst.reshape(128, 512), invc=invc.reshape(128, 512)), cdec


CONST_SHAPES = lambda T: dict(cosT=[128, T], sinT=[128, T], maskT=[128, NH * 128], qdec=[128, NH],
                              kdec=[128, NH], ident=[128, 128], ones=[128, 128], acur=[128, 512],
                              aprev=[128, 512], afirst=[128, 512], invc=[128, 512])


def const_names(kinds):
    names = {"ones"}
    if "ret" in kinds:
        names |= {"cosT", "sinT", "maskT", "qdec", "kdec", "ident"}
    if "pool" in kinds:
        names |= {"acur", "aprev", "afirst", "invc"}
    return names


def vec_layout(v):
    v = np.asarray(v, np.float32).reshape(-1, 128)
    return np.ascontiguousarray(v.T)


class Builder:
    def __init__(self, T, layers):
        self.T = T
        self.NT = T // TT
        self.layers = layers
        nc = bass.Bass("TRN2", target_bir_lowering=False)
        self.nc = nc
        self.P = Prog(nc)
        self.st = contextlib.ExitStack()
        self.tasks = []
        self.uoff = {}
        self.blkctr = {}
        self.UBYTES = 76 * 1024
        self.out_res = set()
        self.nwt = 0
        self.ctr = {}
        _, self.cdec = const_tables(128)

    def rot(self, name, n):
        c = self.ctr.get(name, 0)
        self.ctr[name] = c + 1
        return c % n

    def sb(self, name, shape, dt):
        return self.st.enter_context(self.nc.sbuf_tensor("sb_" + name, shape, dt))

    def carve(self, setname, shape, dt):
        nbytes = int(np.prod(shape[1:])) * (4 if dt == F32 else 2)
        nbytes = (nbytes + 63) // 64 * 64
        off = self.uoff.get(setname, 0)
        self.uoff[setname] = off + nbytes
        assert off + nbytes <= self.UBYTES, (setname, off + nbytes)
        v = self.U[:, off // 2:(off + nbytes) // 2]
        if dt == F32:
            v = v.bitcast(F32)
        n = int(np.prod(shape[1:]))
        v = v[:, 0:n]
        if len(shape) == 3:
            v = v.rearrange("p (a b) -> p a b", a=shape[1])
        elif len(shape) == 4:
            v = v.rearrange("p (a b c) -> p a b c", a=shape[1], b=shape[2])
        return v

    def bank(self):
        i = self.rot("bank", 6)
        return self.psA[i], "psA%d" % i

    def bankT(self):
        i = self.rot("bankT", 2)
        return self.psT[i], "psT%d" % i

    def mm(self, out, ores, lhsT, rhs, reads, start, stop, inc=None):
        if inc is None:
            inc = stop
        self.P.op("pe", lambda e: e.matmul(out, lhsT, rhs, start=start, stop=stop),
                  reads=reads, writes=[ores], inc=inc)

    def tr(self, out, ores, in_, reads, inc):
        ident = self.ident
        self.P.op("pe", lambda e: e.transpose(out, in_, ident[:, :]),
                  reads=list(reads) + ["const"], writes=[ores], inc=inc)

    def add_task(self, load, compute, key=None):
        if load is not None:
            slot = self.nwt % NSLOT
            self.nwt += 1
            if key is not None:
                li, t = key
                blk = self.blkctr.get(key, 0)
                self.blkctr[key] = blk + 1
                wsc = self.wsc[li]
                res = "wsc%d_%d" % (li, blk)
                load_fp32 = load
                if t == 0:
                    def load(slot, load_fp32=load_fp32, wsc=wsc, blk=blk, res=res):
                        load_fp32(slot)
                        self.P.dma("sp", "wb%d" % slot, wsc[blk], self.W[slot][:, :],
                                   reads=["W%d" % slot], writes=[res])
                else:
                    def load(slot, wsc=wsc, blk=blk, res=res):
                        self.P.dma("sp", "w%d" % slot, self.W[slot][:, :], wsc[blk],
                                   reads=[res], writes=["W%d" % slot])
        else:
            slot = None
        self.tasks.append((load, compute, slot))

    def wload(self, slot, dst_view, src):
        self.P.dma("pool", "w%d" % slot, dst_view, src, writes=["W%d" % slot])

    def build(self):
        nc, T = self.nc, self.T
        dram_in = lambda name, shape: nc.dram_tensor(name, shape, F32, kind="ExternalInput").ap()
        self.hin = dram_in("hin", [D, T])
        self.hout = nc.dram_tensor("hout", [D, T], F32, kind="ExternalOutput").ap()
        self.hbuf = nc.dram_tensor("hbuf", [D, T], F32).ap() if len(self.layers) > 1 else None
        self.cd = {k: dram_in(k, s) for k, s in CONST_SHAPES(T).items() if k in const_names(self.layers)}
        self.lw = []
        for li, kind in enumerate(self.layers):
            p = {}
            if kind == "ret":
                p["norm"] = dram_in("l%d_norm" % li, [128, 16])
                p["w_in"] = dram_in("l%d_w_in" % li, [D, 12288])
                p["gn"] = dram_in("l%d_gn" % li, [128, 32])
                p["w_out"] = dram_in("l%d_w_out" % li, [4096, D])
            elif kind == "pool":
                p["norm"] = dram_in("l%d_norm" % li, [128, 16])
                p["w_in"] = dram_in("l%d_w_in" % li, [D, 8192])
                p["w_grp"] = dram_in("l%d_w_grp" % li, [4, 1024, 1024])
                p["b"] = dram_in("l%d_b" % li, [128, 32])
                p["s"] = dram_in("l%d_s" % li, [128, 32])
                p["w_out"] = dram_in("l%d_w_out" % li, [4096, D])
            else:
                p["norm"] = dram_in("l%d_norm" % li, [128, 16])
            self.lw.append(p)

        self.wsc = {}
        for li, kind in enumerate(self.layers):
            if kind in ("ret", "pool") and self.NT > 1:
                nblk = 32 if kind == "ret" else 28
                self.wsc[li] = nc.dram_tensor("wsc%d" % li, [nblk, 128, 8192], BF16).ap()
        sb = self.sb
        has_ret = "ret" in self.layers
        has_pool = "pool" in self.layers
        self.W = [sb("W%d" % i, [128, 8192], BF16) for i in range(NSLOT)]
        self.hs = [sb("hs%d" % i, [128, TT], F32) for i in range(4)]
        self.hnew = [sb("hnew%d" % i, [128, TT], F32) for i in range(2)]
        self.hn = sb("hn", [128, KC, TT], BF16)
        self.sq = [sb("sq%d" % i, [128, TT], BF16) for i in range(2)]
        self.rt = sb("rt", [128, TT], F32)
        self.rstd = sb("rstd", [128, TT], F32)
        self.gT = sb("gT", [128, 32, TT], BF16)
        self.ident = sb("ident", [128, 128], BF16)
        self.ones = sb("ones", [128, 128], BF16)
        self.epsb = sb("epsb", [128, 1], F32)
        self.vec = [sb("vec%d" % li, [128, 112], F32) for li in range(len(self.layers))]
        self.U = sb("U", [128, self.UBYTES // 2], BF16)
        if has_ret:
            cv = lambda shape, dt: self.carve("ret", shape, dt)
            self.mask = sb("mask", [128, NH, 128], F32)
            self.qdec = sb("qdec", [128, NH], F32)
            self.kdec = sb("kdec", [128, NH], F32)
            self.cos = cv([128, TT], F32)
            self.sin = cv([128, TT], F32)
            self.S = cv([128, NH, 2, 512], F32)
            self.Sbf = [cv([128, 2, 512], BF16) for i in range(2)]
            self.qT = [cv([128, 2, TT], BF16)]
            self.kT = [cv([128, 2, TT], BF16)]
            self.v = [cv([128, 4, 512], BF16)]
            self.kd = [cv([128, 256], BF16) for i in range(2)]
            self.sm = [cv([128, 128], BF16) for i in range(2)]
            self.oraw4 = cv([128, 4, 512], F32)
            self.mv4 = cv([128, 4, 2], F32)
            self.sd4 = cv([128, 4], F32)
            self.rs4 = cv([128, 4], F32)
            self.on = [cv([128, 4, 512], BF16)]
            self.sz = [cv([128, TT], F32) for i in range(2)]
            self.rtmp = [cv([128, TT], F32) for i in range(4)]
            self.st6 = [cv([128, 6], F32) for i in range(4)]
        if has_pool:
            cv = lambda shape, dt: self.carve("pool", shape, dt)
            self.acur = sb("acur", [128, 4, 128], BF16)
            self.aprev = sb("aprev", [128, 4, 128], BF16)
            self.afirst = sb("afirst", [128, 4, 128], BF16)
            self.invc = sb("invc", [128, 4, 128], F32)
            self.bs = [sb("bs%d" % li, [128, 32], F32) for li in range(len(self.layers))]
            self.u = cv([128, 4, 4096], BF16)
            self.uprev = cv([128, 4096], BF16)
            self.mixs = [cv([128, 8, TT], BF16) for i in range(2)]
            self.m2 = [cv([128, TT], F32) for i in range(2)]
        self.psA = [self.st.enter_context(nc.psum_tensor("psA%d" % i, [128, 512], F32)) for i in range(6)]
        self.psT = [self.st.enter_context(nc.psum_tensor("psT%d" % i, [128, 1024], BF16)) for i in range(2)]

        P = self.P
        if has_ret:
            P.dma("pool", "c0", self.ident[:, :], self.cd["ident"][:, :], writes=["const"])
        P.dma("pool", "c1", self.ones[:, :], self.cd["ones"][:, :], writes=["const1"])
        epsb = self.epsb
        P.op("dve", lambda e: e.memset(epsb[:, :], EPS), writes=["epsb"])
        if has_ret:
            P.dma("sp", "c2", self.mask[:, :, :], self.cd["maskT"].rearrange("p (h i) -> p h i", h=NH), writes=["mask"])
            P.dma("sp", "c3", self.qdec[:, :], self.cd["qdec"][:, :], writes=["qdec"])
            P.dma("sp", "c4", self.kdec[:, :], self.cd["kdec"][:, :], writes=["kdec"])
        if has_pool:
            for nm, key in (("acur", "c5"), ("aprev", "c6"), ("afirst", "c7")):
                P.dma("pool", key, getattr(self, nm)[:, :, :],
                      self.cd[nm].rearrange("p (g t) -> p g t", g=4), writes=[nm])
            P.dma("sp", "c8", self.invc[:, :, :], self.cd["invc"].rearrange("p (g t) -> p g t", g=4), writes=["invc"])
        for li, kind in enumerate(self.layers):
            p = self.lw[li]
            vec = self.vec[li]
            P.dma("sp", "v%d" % li, vec[:, 0:16], p["norm"][:, :], writes=["vec%d" % li])
            if kind == "ret":
                P.dma("sp", "v%d" % li, vec[:, 16:48], p["gn"][:, :], writes=["vec%d" % li])
            elif kind == "pool":
                P.dma("sp", "v%d" % li, vec[:, 16:48], p["b"][:, :], writes=["vec%d" % li])
                P.dma("sp", "v%d" % li, vec[:, 48:80], p["s"][:, :], writes=["vec%d" % li])
                bs = self.bs[li]
                self._tt("dve", bs[:, :], vec[:, 16:48], vec[:, 48:80], ALU.mult,
                         ["vec%d" % li], ["bs%d" % li])

        nl = len(self.layers)
        for li, kind in enumerate(self.layers):
            src = self.hin if li == 0 else self.hbuf
            dst = self.hout if li == nl - 1 else self.hbuf
            if li > 0:
                self.add_task(None, lambda slot: self.P.fence())
            if kind == "ret":
                self.ret_layer(li, src, dst)
            elif kind == "pool":
                self.pool_layer(li, src, dst)
            else:
                self.fnorm_layer(li, src, dst)

        wt = [i for i, t in enumerate(self.tasks) if t[0] is not None]
        nl_issued = 0
        wt_done = 0
        for i, (load, compute, slot) in enumerate(self.tasks):
            while nl_issued < len(wt) and nl_issued < wt_done + NSLOT:
                j = wt[nl_issued]
                self.tasks[j][0](self.tasks[j][2])
                nl_issued += 1
            compute(slot)
            if load is not None:
                wt_done += 1
        P.wait_all("sp", sorted(self.out_res))
        P.emit()
        self.st.close()
        return nc

    def _tt(self, eng, out, in0, in1, op, reads, writes):
        self.P.op(eng, lambda e: e.tensor_tensor(out, in0, in1, op), reads=reads, writes=writes)

    def _ts(self, eng, out, in0, s1, s2, op0, op1, reads, writes):
        if s2 is None:
            self.P.op(eng, lambda e: e.tensor_scalar(out, in0, s1, None, op0), reads=reads, writes=writes)
        else:
            self.P.op(eng, lambda e: e.tensor_scalar(out, in0, s1, s2, op0, op1), reads=reads, writes=writes)

    def _stt(self, eng, out, in0, scalar, in1, op0, op1, reads, writes):
        self.P.op(eng, lambda e: e.scalar_tensor_tensor(out, in0, scalar, in1, op0, op1),
                  reads=reads, writes=writes)

    def _act(self, out, in_, func, reads, writes, bias=None, scale=None):
        kw = {}
        if bias is not None:
            kw["bias"] = bias
        if scale is not None:
            kw["scale"] = scale
        self.P.op("act", lambda e: e.activation(out, in_, func, **kw), reads=reads, writes=writes)

    def _copy(self, eng, out, in_, reads, writes):
        self.P.op(eng, lambda e: e.tensor_copy(out, in_), reads=reads, writes=writes)

    def norm_stage(self, li, src, t, final_dst=None):
        P = self.P
        vec = self.vec[li]
        cols = slice(t * TT, (t + 1) * TT)
        srcv = src.rearrange("(k p) t -> p k t", p=128)
        ps, pres = self.bank()
        for kc in range(KC):
            s = self.rot("hs", 4)
            P.dma("sp", "hs%d" % s, self.hs[s][:, :], srcv[:, kc, cols], reads=["h%d_%d" % (t, kc)], writes=["hs%d" % s])
            q = self.rot("sq", 2)
            self._act(self.sq[q][:, :], self.hs[s][:, :], AF.Square, ["hs%d" % s], ["sq%d" % q])
            self.mm(ps[:, :], pres, self.ones[:, :], self.sq[q][:, :], ["const1", "sq%d" % q],
                    kc == 0, kc == KC - 1, inc=True)
        self._ts("dve", self.rt[:, :], ps[:, :], 1.0 / D, EPS, ALU.mult, ALU.add, [pres], ["rt"])
        self._act(self.rt[:, :], self.rt[:, :], AF.Sqrt, ["rt"], ["rt"])
        rstd, rt = self.rstd, self.rt
        P.op("dve", lambda e: e.reciprocal(rstd[:, :], rt[:, :]), reads=["rt"], writes=["rstd"])
        for kc in range(KC):
            s = self.rot("hs", 4)
            P.dma("sp", "hs%d" % s, self.hs[s][:, :], srcv[:, kc, cols], reads=["h%d_%d" % (t, kc)], writes=["hs%d" % s])
            if final_dst is None:
                self._stt("dve", self.hn[:, kc, :], self.hs[s][:, :], vec[:, kc:kc + 1], self.rstd[:, :],
                          ALU.mult, ALU.mult, ["hs%d" % s, "rstd", "vec%d" % li], ["hn%d" % kc])
            else:
                o = self.rot("hnew", 2)
                self._stt("dve", self.hnew[o][:, :], self.hs[s][:, :], vec[:, kc:kc + 1], self.rstd[:, :],
                          ALU.mult, ALU.mult, ["hs%d" % s, "rstd", "vec%d" % li], ["hnew%d" % o])
                dstv = final_dst.rearrange("(k p) t -> p k t", p=128)
                P.dma("sp", "ho%d" % o, dstv[:, kc, cols], self.hnew[o][:, :],
                      reads=["hnew%d" % o], writes=["f%d_%d" % (t, kc)])
                self.out_res.add("f%d_%d" % (t, kc))

    def fnorm_layer(self, li, src, dst):
        for t in range(self.NT):
            self.add_task(None, lambda slot, t=t: self.norm_stage(li, src, t, final_dst=dst))

    def out_tasks(self, li, src, dst, t):
        P = self.P
        w_out = self.lw[li]["w_out"].rearrange("(k p) c -> p k c", p=128)
        cols = slice(t * TT, (t + 1) * TT)
        srcv = src.rearrange("(k p) t -> p k t", p=128)
        dstv = dst.rearrange("(k p) t -> p k t", p=128)
        for j in range(8):
            def load(slot, j=j):
                Wv = self.W[slot][:, :].rearrange("p (k c) -> p k c", k=32)
                self.wload(slot, Wv, w_out[:, :, j * 256:(j + 1) * 256])

            def compute(slot, j=j):
                Wv = self.W[slot][:, :].rearrange("p (k c) -> p k c", k=32)
                for dl in range(2):
                    dc = j * 2 + dl
                    ps, pres = self.bank()
                    for kc in range(32):
                        self.mm(ps[:, :], pres, Wv[:, kc, dl * 128:(dl + 1) * 128], self.gT[:, kc, :],
                                ["W%d" % slot, "gT%d" % kc], kc == 0, kc == 31)
                    s = self.rot("hs", 4)
                    hres = "h%d_%d" % (t, dc)
                    P.dma("sp", "hs%d" % s, self.hs[s][:, :], srcv[:, dc, cols], reads=[hres], writes=["hs%d" % s])
                    o = self.rot("hnew", 2)
                    self._tt("dve", self.hnew[o][:, :], ps[:, :], self.hs[s][:, :], ALU.add,
                             [pres, "hs%d" % s], ["hnew%d" % o])
                    P.dma("sp", "ho%d" % o, dstv[:, dc, cols], self.hnew[o][:, :],
                          reads=["hnew%d" % o], writes=[hres])
                    self.out_res.add(hres)
            self.add_task(load, compute, key=(li, t) if self.NT > 1 else None)

    def pool_layer(self, li, src, dst):
        P = self.P
        p = self.lw[li]
        vec = self.vec[li]
        w_in = p["w_in"].rearrange("(k p) c -> p k c", p=128)
        w_grp = p["w_grp"].rearrange("g (k p) c -> g p k c", p=128)
        uprev = self.uprev
        self.add_task(None, lambda slot: P.op("pool", lambda e: e.memset(uprev[:, :], 0.0), writes=["uprev"]))
        for t in range(self.NT):
            self.add_task(None, lambda slot, t=t: self.norm_stage(li, src, t))
            for j in range(8):
                def load(slot, j=j):
                    Wv = self.W[slot][:, :].rearrange("p (k c) -> p k c", k=16)
                    self.wload(slot, Wv, w_in[:, :, j * 512:(j + 1) * 512])

                def compute(slot, j=j):
                    Wv = self.W[slot][:, :].rearrange("p (k c) -> p k c", k=16)
                    for tc in range(4):
                        ps, pres = self.bank()
                        for kc in range(KC):
                            self.mm(ps[:, :], pres, self.hn[:, kc, tc * 128:(tc + 1) * 128], Wv[:, kc, :],
                                    ["hn%d" % kc, "W%d" % slot], kc == 0, kc == KC - 1)
                        dstu = self.u[:, tc, j * 512:(j + 1) * 512]
                        if (tc + j) % 2 == 0:
                            self._act(dstu, ps[:, :], AF.Copy, [pres], ["u%d_%d" % (tc, j)])
                        else:
                            self._copy("dve", dstu, ps[:, :], [pres], ["u%d_%d" % (tc, j)])
                self.add_task(load, compute, key=(li, t) if self.NT > 1 else None)
            for j in range(8):
                def load(slot, j=j):
                    Wv = self.W[slot][:, :].rearrange("p (k c) -> p k c", k=16)
                    self.wload(slot, Wv, w_in[:, :, 4096 + j * 512:4096 + (j + 1) * 512])

                def compute(slot, j=j):
                    Wv = self.W[slot][:, :].rearrange("p (k c) -> p k c", k=16)
                    for q in range(4):
                        oc = j * 4 + q
                        ps, pres = self.bank()
                        for kc in range(KC):
                            self.mm(ps[:, :], pres, Wv[:, kc, q * 128:(q + 1) * 128], self.hn[:, kc, :],
                                    ["W%d" % slot, "hn%d" % kc], kc == 0, kc == KC - 1)
                        self._act(self.gT[:, oc, :], ps[:, :], AF.Silu, [pres], ["gT%d" % oc])
                self.add_task(load, compute, key=(li, t) if self.NT > 1 else None)
            for g in range(4):
                def load(slot, g=g):
                    Wv = self.W[slot][:, :].rearrange("p (k c) -> p k c", k=8)
                    self.wload(slot, Wv, w_grp[g])

                def compute(slot, g=g, t=t):
                    Wv = self.W[slot][:, :].rearrange("p (k c) -> p k c", k=8)
                    mx = self.mixs[g % 2]
                    mres = "mixs%d" % (g % 2)
                    j0 = g * 2
                    for cl in range(8):
                        cc = g * 8 + cl
                        ublk = cc // 4
                        ps, pres = self.bank()
                        for tc in range(4):
                            first = (t == 0 and tc == 0)
                            A = (self.afirst if first else self.acur)
                            ares = "afirst" if first else "acur"
                            cur = self.u[:, tc, cc * 128:(cc + 1) * 128]
                            if tc == 0:
                                prv = self.uprev[:, cc * 128:(cc + 1) * 128]
                                prv_res = "uprev"
                            else:
                                prv = self.u[:, tc - 1, cc * 128:(cc + 1) * 128]
                                prv_res = "u%d_%d" % (tc - 1, ublk)
                            o = ps[:, tc * 128:(tc + 1) * 128]
                            self.mm(o, pres, cur, A[:, g, :], ["u%d_%d" % (tc, ublk), ares], True, False, inc=False)
                            self.mm(o, pres, prv, self.aprev[:, g, :], [prv_res, "aprev"], False, True,
                                    inc=(tc == 3))
                        if t == 0:
                            self._tt("dve", mx[:, cl, 0:128], ps[:, 0:128], self.invc[:, g, :], ALU.mult,
                                     [pres, "invc"], [mres + "_%d" % cl])
                            self._ts("dve", mx[:, cl, 128:TT], ps[:, 128:TT], 1.0 / WIN[g], None, ALU.mult, None,
                                     [pres], [mres + "_%db" % cl])
                        else:
                            self._ts("dve", mx[:, cl, :], ps[:, :], 1.0 / WIN[g], None, ALU.mult, None,
                                     [pres], [mres + "_%d" % cl, mres + "_%db" % cl])
                    for jd in range(8):
                        oc = g * 8 + jd
                        ps, pres = self.bank()
                        for kc in range(8):
                            self.mm(ps[:, :], pres, Wv[:, kc, jd * 128:(jd + 1) * 128], mx[:, kc, :],
                                    ["W%d" % slot, mres + "_%d" % kc, mres + "_%db" % kc], kc == 0, kc == 7)
                        m = self.rot("m2", 2)
                        self._act(self.m2[m][:, :], ps[:, :], AF.Identity, [pres, "vec%d" % li, "bs%d" % li],
                                  ["m2_%d" % m], bias=self.bs[li][:, oc:oc + 1], scale=vec[:, 48 + oc:49 + oc])
                        eng = "dve" if jd % 2 == 0 else "pool"
                        self._tt(eng, self.gT[:, oc, :], self.m2[m][:, :], self.gT[:, oc, :], ALU.mult,
                                 ["m2_%d" % m, "gT%d" % oc], ["gT%d" % oc])
                    if g == 3:
                        ures = ["u3_%d" % b for b in range(8)]
                        self._copy("pool", self.uprev[:, :], self.u[:, 3, :], ures, ["uprev"])
                self.add_task(load, compute, key=(li, t) if self.NT > 1 else None)
            self.out_tasks(li, src, dst, t)

    def ret_layer(self, li, src, dst):
        P = self.P
        p = self.lw[li]
        vec = self.vec[li]
        w_in = p["w_in"].rearrange("(k p) c -> p k c", p=128)
        S = self.S
        self.add_task(None, lambda slot: P.op("pool", lambda e: e.memset(S[:, :, :, :], 0.0),
                                              writes=["S%d" % h for h in range(NH)]))
        for t in range(self.NT):
            def pre(slot, t=t):
                self.norm_stage(li, src, t)
                cols = slice(t * TT, (t + 1) * TT)
                P.dma("sp", "cos", self.cos[:, :], self.cd["cosT"][:, cols], writes=["cos"])
                P.dma("sp", "sin", self.sin[:, :], self.cd["sinT"][:, cols], writes=["sin"])
            self.add_task(None, pre)
            for h in range(NH):
                hb = 0

                def load_qk(slot, h=h):
                    Wv = self.W[slot][:, :].rearrange("p (k c) -> p k c", k=16)
                    self.wload(slot, Wv[:, :, 0:256], w_in[:, :, h * 256:(h + 1) * 256])
                    self.wload(slot, Wv[:, :, 256:512], w_in[:, :, 2048 + h * 256:2048 + (h + 1) * 256])

                def comp_qk(slot, h=h, hb=hb):
                    Wv = self.W[slot][:, :].rearrange("p (k c) -> p k c", k=16)
                    for wi, (dstT, dres) in enumerate(((self.qT[hb], "qT%d" % hb), (self.kT[hb], "kT%d" % hb))):
                        pa, para = self.bank()
                        pb, parb = self.bank()
                        for half, (ps, pres) in enumerate(((pa, para), (pb, parb))):
                            c0 = wi * 256 + half * 128
                            for kc in range(KC):
                                self.mm(ps[:, :], pres, Wv[:, kc, c0:c0 + 128], self.hn[:, kc, :],
                                        ["W%d" % slot, "hn%d" % kc], kc == 0, kc == KC - 1)
                        r = self.rtmp
                        self._tt("dve", r[0][:, :], pa[:, :], self.cos[:, :], ALU.mult, [para, "cos"], ["rtmp0"])
                        self._tt("dve", r[1][:, :], pb[:, :], self.sin[:, :], ALU.mult, [parb, "sin"], ["rtmp1"])
                        self._tt("pool", dstT[:, 0, :], r[0][:, :], r[1][:, :], ALU.subtract,
                                 ["rtmp0", "rtmp1"], [dres])
                        self._tt("dve", r[2][:, :], pa[:, :], self.sin[:, :], ALU.mult, [para, "sin"], ["rtmp2"])
                        self._tt("dve", r[3][:, :], pb[:, :], self.cos[:, :], ALU.mult, [parb, "cos"], ["rtmp3"])
                        self._tt("pool", dstT[:, 1, :], r[2][:, :], r[3][:, :], ALU.add,
                                 ["rtmp2", "rtmp3"], [dres])
                self.add_task(load_qk, comp_qk, key=(li, t) if self.NT > 1 else None)

                def load_v(slot, h=h):
                    Wv = self.W[slot][:, :].rearrange("p (k c) -> p k c", k=16)
                    self.wload(slot, Wv, w_in[:, :, 4096 + h * 512:4096 + (h + 1) * 512])

                def comp_v(slot, h=h, hb=hb, t=t):
                    Wv = self.W[slot][:, :].rearrange("p (k c) -> p k c", k=16)
                    vb, vres = self.v[hb], "v%d" % hb
                    for tc in range(4):
                        ps, pres = self.bank()
                        for kc in range(KC):
                            self.mm(ps[:, :], pres, self.hn[:, kc, tc * 128:(tc + 1) * 128], Wv[:, kc, :],
                                    ["hn%d" % kc, "W%d" % slot], kc == 0, kc == KC - 1)
                        self._act(vb[:, tc, :], ps[:, :], AF.Copy, [pres], [vres + "_%d" % tc])
                    self.ret_core(li, h, hb)
                self.add_task(load_v, comp_v, key=(li, t) if self.NT > 1 else None)

                def load_z(slot, h=h):
                    Wv = self.W[slot][:, :].rearrange("p (k c) -> p k c", k=16)
                    self.wload(slot, Wv, w_in[:, :, 8192 + h * 512:8192 + (h + 1) * 512])

                def comp_z(slot, h=h, hb=hb):
                    Wv = self.W[slot][:, :].rearrange("p (k c) -> p k c", k=16)
                    onb, onres = self.on[hb], "on%d" % hb
                    for ec in range(4):
                        oc = h * 4 + ec
                        ps, pres = self.bank()
                        for kc in range(KC):
                            self.mm(ps[:, :], pres, Wv[:, kc, ec * 128:(ec + 1) * 128], self.hn[:, kc, :],
                                    ["W%d" % slot, "hn%d" % kc], kc == 0, kc == KC - 1)
                        z = self.rot("sz", 2)
                        self._act(self.sz[z][:, :], ps[:, :], AF.Silu, [pres], ["sz%d" % z])
                        pT, ptres = self.bankT()
                        for tc in range(4):
                            self.tr(pT[:, tc * 128:(tc + 1) * 128], ptres, onb[:, tc, ec * 128:(ec + 1) * 128],
                                    [onres + "_%d" % tc], inc=(tc == 3))
                        self._stt("dve", self.gT[:, oc, :], pT[:, 0:TT], vec[:, 16 + oc:17 + oc], self.sz[z][:, :],
                                  ALU.mult, ALU.mult, [ptres, "vec%d" % li, "sz%d" % z], ["gT%d" % oc])
                self.add_task(load_z, comp_z, key=(li, t) if self.NT > 1 else None)
            self.out_tasks(li, src, dst, t)

    def ret_core(self, li, h, hb):
        P = self.P
        qT, kT, vb, onb = self.qT[hb], self.kT[hb], self.v[hb], self.on[hb]
        qres, kres, vres, onres = "qT%d" % hb, "kT%d" % hb, "v%d" % hb, "on%d" % hb
        S = self.S
        Sres = "S%d" % h
        g128 = self.cdec[h]
        for dch in range(2):
            self._copy("pool", self.Sbf[0][:, dch, :], S[:, h, dch, :], [Sres], ["Sbf0_%d" % dch])
        for tc in range(4):
            par = tc % 2
            tcs = slice(tc * 128, (tc + 1) * 128)
            pT, ptres = self.bankT()
            for dch in range(2):
                self.tr(pT[:, dch * 128:(dch + 1) * 128], ptres, kT[:, dch, tcs], [kres], inc=(dch == 1))
            x = self.rot("kd", 2)
            self._act(self.kd[x][:, :], pT[:, 0:256], AF.Identity, [ptres, "kdec"], ["kd%d" % x],
                      scale=self.kdec[:, h:h + 1])
            pss, psres = self.bank()
            for dch in range(2):
                self.mm(pss[:, 0:128], psres, kT[:, dch, tcs], qT[:, dch, tcs], [kres, qres], dch == 0, dch == 1)
            y = self.rot("sm", 2)
            self._tt("dve", self.sm[y][:, :], pss[:, 0:128], self.mask[:, h, :], ALU.mult,
                     [psres, "mask"], ["sm%d" % y])
            psS = []
            for dch in range(2):
                ps, pres = self.bank()
                self.mm(ps[:, :], pres, self.kd[x][:, dch * 128:(dch + 1) * 128], vb[:, tc, :],
                        ["kd%d" % x, vres + "_%d" % tc], True, True)
                psS.append((ps, pres))
            pso, pores = self.bank()
            self.mm(pso[:, :], pores, self.sm[y][:, :], vb[:, tc, :], ["sm%d" % y, vres + "_%d" % tc], True, False,
                    inc=False)
            for dch in range(2):
                self.mm(pso[:, :], pores, qT[:, dch, tcs], self.Sbf[par][:, dch, :],
                        [qres, "Sbf%d_%d" % (par, dch)], False, dch == 1)
            for dch in range(2):
                ps, pres = psS[dch]
                self._stt("dve", S[:, h, dch, :], S[:, h, dch, :], g128, ps[:, :], ALU.mult, ALU.add,
                          [Sres, pres], [Sres])
                if tc < 3:
                    self._copy("pool", self.Sbf[1 - par][:, dch, :], S[:, h, dch, :], [Sres],
                               ["Sbf%d_%d" % (1 - par, dch)])
            orw = self.oraw4
            self._act(orw[:, tc, :], pso[:, :], AF.Identity, [pores, "qdec"], ["oraw_%d" % tc],
                      scale=self.qdec[:, h:h + 1])
            m = self.rot("stat", 4)
            st6, mv4 = self.st6[m], self.mv4
            P.op("dve", lambda e, st6=st6, tc=tc: e.bn_stats(st6[:, :], orw[:, tc, :]),
                 reads=["oraw_%d" % tc], writes=["st6_%d" % m])
            P.op("dve", lambda e, st6=st6, tc=tc: e.bn_aggr(mv4[:, tc, :], st6[:, :]),
                 reads=["st6_%d" % m], writes=["mv4_%d" % tc])
        sd4, rs4, mv4, orw = self.sd4, self.rs4, self.mv4, self.oraw4
        mvr = ["mv4_%d" % tc for tc in range(4)]
        self._act(sd4[:, :], mv4[:, :, 1], AF.Sqrt, mvr + ["epsb"], ["sd4"], bias=self.epsb[:, 0:1], scale=1.0)
        P.op("dve", lambda e: e.reciprocal(rs4[:, :], sd4[:, :]), reads=["sd4"], writes=["rs4"])
        for tc in range(4):
            self._ts("pool", onb[:, tc, :], orw[:, tc, :], mv4[:, tc, 0:1], rs4[:, tc:tc + 1], ALU.subtract, ALU.mult,
                     ["oraw_%d" % tc, "mv4_%d" % tc, "rs4"], [onres + "_%d" % tc])


_CACHE = {}


def get_prog(T, layers):
    key = (T, tuple(layers))
    if key not in _CACHE:
        _CACHE[key] = Builder(T, list(layers)).build()
    return _CACHE[key]


def layer_inputs(li, kind, j, inp):
    d = {}
    if kind == "ret":
        d["l%d_norm" % li] = vec_layout(inp["ret_norm"][j])
        d["l%d_w_in" % li] = np.ascontiguousarray(inp["ret_w_in"][j], dtype=np.float32)
        d["l%d_gn" % li] = vec_layout(inp["ret_gn"][j])
        d["l%d_w_out" % li] = np.ascontiguousarray(inp["ret_w_out"][j], dtype=np.float32)
    elif kind == "pool":
        d["l%d_norm" % li] = vec_layout(inp["pool_norm"][j])
        d["l%d_w_in" % li] = np.ascontiguousarray(inp["pool_w_in"][j], dtype=np.float32)
        d["l%d_w_grp" % li] = np.ascontiguousarray(inp["pool_w_grp"][j], dtype=np.float32)
        d["l%d_b" % li] = vec_layout(np.asarray(inp["pool_b_grp"][j]).reshape(-1))
        d["l%d_s" % li] = vec_layout(inp["pool_scale"][j])
        d["l%d_w_out" % li] = np.ascontiguousarray(inp["pool_w_out"][j], dtype=np.float32)
    else:
        d["l%d_norm" % li] = vec_layout(inp["final_norm"])
    return d


FUSED = True


def kernel(**inputs):
    inp = {k: np.asarray(v) for k, v in inputs.items()}
    x = inp["x"].astype(np.float32, copy=False)
    B, S, _ = x.shape
    consts, _ = const_tables(S)
    hT = [np.ascontiguousarray(x[b].T) for b in range(B)]
    cores = list(range(B))
    plan = [("ret", 0), ("pool", 0), ("ret", 1), ("pool", 1), ("fnorm", 0)]
    if FUSED:
        stages = [plan]
    else:
        stages = [[s] for s in plan]
    for stage in stages:
        kinds = [k for k, _ in stage]
        nc = get_prog(S, kinds)
        shared = {k: v for k, v in consts.items() if k in const_names(kinds)}
        for li, (kind, j) in enumerate(stage):
            shared.update(layer_inputs(li, kind, j, inp))
        in_maps = []
        for b in range(B):
            m = dict(shared)
            m["hin"] = hT[b]
            in_maps.append(m)
        res = run_bass_kernel_spmd(nc, in_maps, core_ids=cores)
        hT = [np.asarray(res.results[b]["hout"]) for b in range(B)]
    out = np.stack([h.T for h in hT], axis=0).astype(np.float32)
    return out
```

```python
import contextlib
import numpy as np
import concourse.bass as bass
import concourse.mybir as mybir
from concourse.bass_utils import run_bass_kernel_spmd

F32 = mybir.dt.float32
BF16 = mybir.dt.bfloat16
AF = mybir.ActivationFunctionType
ALU = mybir.AluOpType

D = 2048
KC = 16
TT = 512
NH = 8
EPS = 1e-6
SEQ = 4096
BATCH = 4
NSLOT = 3
WIN = (2, 4, 8, 16)

ENGS = ("pe", "dve", "act", "pool", "sp")


class Prog:
    def __init__(self, nc):
        self.nc = nc
        self.streams = {e: [] for e in ENGS}
        self.count = {}
        self.known = {e: {} for e in ENGS}
        self.lastw = {}
        self.readers = {}
        self.dma_keys = set()

    def _deps(self, eng, reads, writes):
        deps = {}

        def need(kv, same_ok):
            if kv is None:
                return
            k, v = kv
            if k == eng and same_ok:
                return
            if deps.get(k, 0) < v:
                deps[k] = v

        for r in reads:
            need(self.lastw.get(r), same_ok=(eng == "pe"))
        for w in writes:
            need(self.lastw.get(w), same_ok=True)
            for k, v in self.readers.get(w, {}).items():
                need((k, v), same_ok=True)
        kn = self.known[eng]
        out = []
        for k, v in deps.items():
            if kn.get(k, 0) < v:
                kn[k] = v
                out.append((k, v))
        return out

    def _commit(self, key, val, reads, writes):
        for r in reads:
            self.readers.setdefault(r, {})[key] = val
        for w in writes:
            self.lastw[w] = (key, val)
            self.readers[w] = {}

    def op(self, eng, fn, reads=(), writes=(), inc=True):
        waits = self._deps(eng, reads, writes)
        cur = self.count.get(eng, 0)
        if inc:
            self.count[eng] = cur + 1
        self.streams[eng].append((waits, fn, eng, 1 if inc else 0))
        self._commit(eng, cur + 1, reads, writes)

    def dma(self, q, dsem, out, in_, reads=(), writes=()):
        key = "d:" + dsem
        self.dma_keys.add(key)
        waits = self._deps(q, reads, writes)
        self.count[key] = self.count.get(key, 0) + 16
        self.streams[q].append(
            (waits, lambda e: e.dma_start(out=out, in_=in_), key, 16))
        self._commit(key, self.count[key], reads, writes)

    def dmaop(self, q, dsem, fn, reads=(), writes=()):
        key = "d:" + dsem
        self.dma_keys.add(key)
        waits = self._deps(q, reads, writes)
        self.count[key] = self.count.get(key, 0) + 16
        self.streams[q].append((waits, fn, key, 16))
        self._commit(key, self.count[key], reads, writes)

    def fence(self):
        for eng in ENGS:
            waits = []
            for k, v in self.count.items():
                if k == eng:
                    continue
                if self.known[eng].get(k, 0) < v:
                    self.known[eng][k] = v
                    waits.append((k, v))
            self.streams[eng].append((waits, None, None, 0))

    def wait_all(self, eng, res_list):
        waits = self._deps(eng, res_list, ())
        self.streams[eng].append((waits, None, None, 0))

    def emit(self):
        nc = self.nc
        keys = [e for e in ENGS if e != "sp"] + sorted(self.dma_keys)
        with contextlib.ExitStack() as st:
            sems = {k: st.enter_context(nc.semaphore("s_" + k.replace(":", "_")))
                    for k in keys}
            block = st.enter_context(nc.Block())

            def run(e, stream):
                for waits, fn, key, inc in stream:
                    for k, v in waits:
                        e.wait_ge(sems[k], v)
                    if fn is not None:
                        ins = fn(e)
                        if inc:
                            ins.then_inc(sems[key], inc)

            @block.tensor
            def _(e):
                run(e, self.streams["pe"])

            @block.vector
            def _(e):
                run(e, self.streams["dve"])

            @block.scalar
            def _(e):
                run(e, self.streams["act"])

            @block.gpsimd
            def _(e):
                run(e, self.streams["pool"])

            @block.sync
            def _(e):
                run(e, self.streams["sp"])


def const_tables(T):
    half = 128
    inv = 10000.0 ** (-np.arange(half, dtype=np.float64) / half)
    ang = inv[:, None] * np.arange(T, dtype=np.float64)[None, :]
    cosT = np.cos(ang).astype(np.float32)
    sinT = np.sin(ang).astype(np.float32)
    hh = np.arange(NH, dtype=np.float64)
    lg = np.log1p(-np.exp2(-5.0 - hh))
    idx = np.arange(128, dtype=np.float64)
    tri = (idx[None, :] >= idx[:, None]).astype(np.float64)
    maskT = np.exp(-(idx[:, None, None] + 1.0) * lg[None, :, None]) / 16.0 * tri[:, None, :]
    qdec = np.exp((idx[:, None] + 1.0) * lg[None, :])
    kdec = np.exp((127.0 - idx[:, None]) * lg[None, :]) / 16.0
    cdec = [float(np.exp(128.0 * l)) for l in lg]
    ident = np.eye(128, dtype=np.float32)
    ones = np.ones((128, 128), dtype=np.float32)
    acur = np.zeros((128, 4, 128), np.float32)
    aprev = np.zeros((128, 4, 128), np.float32)
    afirst = np.zeros((128, 4, 128), np.float32)
    invc = np.zeros((128, 4, 128), np.float32)
    for g, w in enumerate(WIN):
        for t in range(128):
            for tp in range(t - w + 1, t + 1):
                if tp >= 0:
                    acur[tp, g, t] += 1.0
                    afirst[tp, g, t] += 1.0
                else:
                    aprev[128 + tp, g, t] += 1.0
            acur[t, g, t] -= float(w)
            cnt = min(t + 1, w)
            afirst[t, g, t] -= float(cnt)
            invc[:, g, t] = 1.0 / cnt
    return dict(cosT=cosT, sinT=sinT, maskT=maskT.astype(np.float32).reshape(128, NH * 128),
                qdec=qdec.astype(np.float32), kdec=kdec.astype(np.float32),
                ident=ident, ones=ones,
                acur=acur.reshape(128, 512), aprev=aprev.reshape(128, 512),
                afirst=afirst.reshape(128, 512), invc=invc.reshape(128, 512)), cdec


CONST_SHAPES = lambda T: dict(cosT=[128, T], sinT=[128, T], maskT=[128, NH * 128], qdec=[128, NH],
                              kdec=[128, NH], ident=[128, 128], ones=[128, 128], acur=[128, 512],
                              aprev=[128, 512], afirst=[128, 512], invc=[128, 512])


def const_names(kinds):
    names = {"ones"}
    if "ret" in kinds:
        names |= {"cosT", "sinT", "maskT", "qdec", "kdec", "ident"}
    if "pool" in kinds:
        names |= {"acur", "aprev", "afirst", "invc"}
    return names


def vec_layout(v):
    v = np.asarray(v, np.float32).reshape(-1, 128)
    return np.ascontiguousarray(v.T)


class Builder:
    def __init__(self, T, layers):
        self.T = T
        self.NT = T // TT
        self.layers = layers
        nc = bass.Bass("TRN2", target_bir_lowering=False)
        self.nc = nc
        self.P = Prog(nc)
        self.st = contextlib.ExitStack()
        self.tasks = []
        self.uoff = {}
        self.blkctr = {}
        self.UBYTES = 76 * 1024
        self.out_res = set()
        self.nwt = 0
        self.ctr = {}
        _, self.cdec = const_tables(128)

    def rot(self, name, n):
        c = self.ctr.get(name, 0)
        self.ctr[name] = c + 1
        return c % n

    def sb(self, name, shape, dt):
        return self.st.enter_context(self.nc.sbuf_tensor("sb_" + name, shape, dt))

    def carve(self, setname, shape, dt):
        nbytes = int(np.prod(shape[1:])) * (4 if dt == F32 else 2)
        nbytes = (nbytes + 63) // 64 * 64
        off = self.uoff.get(setname, 0)
        self.uoff[setname] = off + nbytes
        assert off + nbytes <= self.UBYTES, (setname, off + nbytes)
        v = self.U[:, off // 2:(off + nbytes) // 2]
        if dt == F32:
            v = v.bitcast(F32)
        n = int(np.prod(shape[1:]))
        v = v[:, 0:n]
        if len(shape) == 3:
            v = v.rearrange("p (a b) -> p a b", a=shape[1])
        elif len(shape) == 4:
            v = v.rearrange("p (a b c) -> p a b c", a=shape[1], b=shape[2])
        return v

    def bank(self):
        i = self.rot("bank", 6)
        return self.psA[i], "psA%d" % i

    def bankT(self):
        i = self.rot("bankT", 2)
        return self.psT[i], "psT%d" % i

    def mm(self, out, ores, lhsT, rhs, reads, start, stop, inc=None):
        if inc is None:
            inc = stop
        self.P.op("pe", lambda e: e.matmul(out, lhsT, rhs, start=start, stop=stop),
                  reads=reads, writes=[ores], inc=inc)

    def tr(self, out, ores, in_, reads, inc):
        ident = self.ident
        self.P.op("pe", lambda e: e.transpose(out, in_, ident[:, :]),
                  reads=list(reads) + ["const"], writes=[ores], inc=inc)

    def add_task(self, load, compute, key=None):
        if load is not None:
            slot = self.nwt % NSLOT
            self.nwt += 1
            if key is not None:
                li, t = key
                blk = self.blkctr.get(key, 0)
                self.blkctr[key] = blk + 1
                wsc = self.wsc[li]
                res = "wsc%d_%d" % (li, blk)
                load_fp32 = load
                if t == 0:
                    def load(slot, load_fp32=load_fp32, wsc=wsc, blk=blk, res=res):
                        load_fp32(slot)
                        self.P.dma("sp", "wb%d" % slot, wsc[blk], self.W[slot][:, :],
                                   reads=["W%d" % slot], writes=[res])
                else:
                    def load(slot, wsc=wsc, blk=blk, res=res):
                        self.P.dma("sp", "w%d" % slot, self.W[slot][:, :], wsc[blk],
                                   reads=[res], writes=["W%d" % slot])
        else:
            slot = None
        self.tasks.append((load, compute, slot))

    def wload(self, slot, dst_view, src):
        self.P.dma("pool", "w%d" % slot, dst_view, src, writes=["W%d" % slot])

    def build(self):
        nc, T = self.nc, self.T
        dram_in = lambda name, shape: nc.dram_tensor(name, shape, F32, kind="ExternalInput").ap()
        self.hin = dram_in("hin", [D, T])
        self.hout = nc.dram_tensor("hout", [D, T], F32, kind="ExternalOutput").ap()
        self.hbuf = nc.dram_tensor("hbuf", [D, T], F32).ap() if len(self.layers) > 1 else None
        self.cd = {k: dram_in(k, s) for k, s in CONST_SHAPES(T).items() if k in const_names(self.layers)}
        self.lw = []
        for li, kind in enumerate(self.layers):
            p = {}
            if kind == "ret":
                p["norm"] = dram_in("l%d_norm" % li, [128, 16])
                p["w_in"] = dram_in("l%d_w_in" % li, [D, 12288])
                p["gn"] = dram_in("l%d_gn" % li, [128, 32])
                p["w_out"] = dram_in("l%d_w_out" % li, [4096, D])
            elif kind == "pool":
                p["norm"] = dram_in("l%d_norm" % li, [128, 16])
                p["w_in"] = dram_in("l%d_w_in" % li, [D, 8192])
                p["w_grp"] = dram_in("l%d_w_grp" % li, [4, 1024, 1024])
                p["b"] = dram_in("l%d_b" % li, [128, 32])
                p["s"] = dram_in("l%d_s" % li, [128, 32])
                p["w_out"] = dram_in("l%d_w_out" % li, [4096, D])
            else:
                p["norm"] = dram_in("l%d_norm" % li, [128, 16])
            self.lw.append(p)

        self.wsc = {}
        for li, kind in enumerate(self.layers):
            if kind in ("ret", "pool") and self.NT > 1:
                nblk = 32 if kind == "ret" else 28
                self.wsc[li] = nc.dram_tensor("wsc%d" % li, [nblk, 128, 8192], BF16).ap()
        sb = self.sb
        has_ret = "ret" in self.layers
        has_pool = "pool" in self.layers
        self.W = [sb("W%d" % i, [128, 8192], BF16) for i in range(NSLOT)]
        self.hs = [sb("hs%d" % i, [128, TT], F32) for i in range(4)]
        self.hnew = [sb("hnew%d" % i, [128, TT], F32) for i in range(2)]
        self.hn = sb("hn", [128, KC, TT], BF16)
        self.sq = [sb("sq%d" % i, [128, TT], BF16) for i in range(2)]
        self.rt = sb("rt", [128, TT], F32)
        self.rstd = sb("rstd", [128, TT], F32)
        self.gT = sb("gT", [128, 32, TT], BF16)
        self.ident = sb("ident", [128, 128], BF16)
        self.ones = sb("ones", [128, 128], BF16)
        self.epsb = sb("epsb", [128, 1], F32)
        self.vec = [sb("vec%d" % li, [128, 112], F32) for li in range(len(self.layers))]
        self.U = sb("U", [128, self.UBYTES // 2], BF16)
        if has_ret:
            cv = lambda shape, dt: self.carve("ret", shape, dt)
            self.mask = sb("mask", [128, NH, 128], F32)
            self.qdec = sb("qdec", [128, NH], F32)
            self.kdec = sb("kdec", [128, NH], F32)
            self.cos = cv([128, TT], F32)
            self.sin = cv([128, TT], F32)
            self.S = cv([128, NH, 2, 512], F32)
            self.Sbf = [cv([128, 2, 512], BF16) for i in range(2)]
            self.qT = [cv([128, 2, TT], BF16)]
            self.kT = [cv([128, 2, TT], BF16)]
            self.v = [cv([128, 4, 512], BF16)]
            self.kd = [cv([128, 256], BF16) for i in range(2)]
            self.sm = [cv([128, 128], BF16) for i in range(2)]
            self.oraw4 = cv([128, 4, 512], F32)
            self.mv4 = cv([128, 4, 2], F32)
            self.sd4 = cv([128, 4], F32)
            self.rs4 = cv([128, 4], F32)
            self.on = [cv([128, 4, 512], BF16)]
            self.sz = [cv([128, TT], F32) for i in range(2)]
            self.rtmp = [cv([128, TT], F32) for i in range(4)]
            self.st6 = [cv([128, 6], F32) for i in range(4)]
        if has_pool:
            cv = lambda shape, dt: self.carve("pool", shape, dt)
            self.acur = sb("acur", [128, 4, 128], BF16)
            self.aprev = sb("aprev", [128, 4, 128], BF16)
            self.afirst = sb("afirst", [128, 4, 128], BF16)
            self.invc = sb("invc", [128, 4, 128], F32)
            self.bs = [sb("bs%d" % li, [128, 32], F32) for li in range(len(self.layers))]
            self.u = cv([128, 4, 4096], BF16)
            self.uprev = cv([128, 4096], BF16)
            self.mixs = [cv([128, 8, TT], BF16) for i in range(2)]
            self.m2 = [cv([128, TT], F32) for i in range(2)]
        self.psA = [self.st.enter_context(nc.psum_tensor("psA%d" % i, [128, 512], F32)) for i in range(6)]
        self.psT = [self.st.enter_context(nc.psum_tensor("psT%d" % i, [128, 1024], BF16)) for i in range(2)]

        P = self.P
        if has_ret:
            P.dma("pool", "c0", self.ident[:, :], self.cd["ident"][:, :], writes=["const"])
        P.dma("pool", "c1", self.ones[:, :], self.cd["ones"][:, :], writes=["const1"])
        epsb = self.epsb
        P.op("dve", lambda e: e.memset(epsb[:, :], EPS), writes=["epsb"])
        if has_ret:
            P.dma("sp", "c2", self.mask[:, :, :], self.cd["maskT"].rearrange("p (h i) -> p h i", h=NH), writes=["mask"])
            P.dma("sp", "c3", self.qdec[:, :], self.cd["qdec"][:, :], writes=["qdec"])
            P.dma("sp", "c4", self.kdec[:, :], self.cd["kdec"][:, :], writes=["kdec"])
        if has_pool:
            for nm, key in (("acur", "c5"), ("aprev", "c6"), ("afirst", "c7")):
                P.dma("pool", key, getattr(self, nm)[:, :, :],
                      self.cd[nm].rearrange("p (g t) -> p g t", g=4), writes=[nm])
            P.dma("sp", "c8", self.invc[:, :, :], self.cd["invc"].rearrange("p (g t) -> p g t", g=4), writes=["invc"])
        for li, kind in enumerate(self.layers):
            p = self.lw[li]
            vec = self.vec[li]
            P.dma("sp", "v%d" % li, vec[:, 0:16], p["norm"][:, :], writes=["vec%d" % li])
            if kind == "ret":
                P.dma("sp", "v%d" % li, vec[:, 16:48], p["gn"][:, :], writes=["vec%d" % li])
            elif kind == "pool":
                P.dma("sp", "v%d" % li, vec[:, 16:48], p["b"][:, :], writes=["vec%d" % li])
                P.dma("sp", "v%d" % li, vec[:, 48:80], p["s"][:, :], writes=["vec%d" % li])
                bs = self.bs[li]
                self._tt("dve", bs[:, :], vec[:, 16:48], vec[:, 48:80], ALU.mult,
                         ["vec%d" % li], ["bs%d" % li])

        nl = len(self.layers)
        for li, kind in enumerate(self.layers):
            src = self.hin if li == 0 else self.hbuf
            dst = self.hout if li == nl - 1 else self.hbuf
            if li > 0:
                self.add_task(None, lambda slot: self.P.fence())
            if kind == "ret":
                self.ret_layer(li, src, dst)
            elif kind == "pool":
                self.pool_layer(li, src, dst)
            else:
                self.fnorm_layer(li, src, dst)

        wt = [i for i, t in enumerate(self.tasks) if t[0] is not None]
        nl_issued = 0
        wt_done = 0
        for i, (load, compute, slot) in enumerate(self.tasks):
            while nl_issued < len(wt) and nl_issued < wt_done + NSLOT:
                j = wt[nl_issued]
                self.tasks[j][0](self.tasks[j][2])
                nl_issued += 1
            compute(slot)
            if load is not None:
                wt_done += 1
        P.wait_all("sp", sorted(self.out_res))
        P.emit()
        self.st.close()
        return nc

    def _tt(self, eng, out, in0, in1, op, reads, writes):
        self.P.op(eng, lambda e: e.tensor_tensor(out, in0, in1, op), reads=reads, writes=writes)

    def _ts(self, eng, out, in0, s1, s2, op0, op1, reads, writes):
        if s2 is None:
            self.P.op(eng, lambda e: e.tensor_scalar(out, in0, s1, None, op0), reads=reads, writes=writes)
        else:
            self.P.op(eng, lambda e: e.tensor_scalar(out, in0, s1, s2, op0, op1), reads=reads, writes=writes)

    def _stt(self, eng, out, in0, scalar, in1, op0, op1, reads, writes):
        self.P.op(eng, lambda e: e.scalar_tensor_tensor(out, in0, scalar, in1, op0, op1),
                  reads=reads, writes=writes)

    def _act(self, out, in_, func, reads, writes, bias=None, scale=None):
        kw = {}
        if bias is not None:
            kw["bias"] = bias
        if scale is not None:
            kw["scale"] = scale
        self.P.op("act", lambda e: e.activation(out, in_, func, **kw), reads=reads, writes=writes)

    def _copy(self, eng, out, in_, reads, writes):
        self.P.op(eng, lambda e: e.tensor_copy(out, in_), reads=reads, writes=writes)

    def norm_stage(self, li, src, t, final_dst=None):
        P = self.P
        vec = self.vec[li]
        cols = slice(t * TT, (t + 1) * TT)
        srcv = src.rearrange("(k p) t -> p k t", p=128)
        ps, pres = self.bank()
        for kc in range(KC):
            s = self.rot("hs", 4)
            P.dma("sp", "hs%d" % s, self.hs[s][:, :], srcv[:, kc, cols], reads=["h%d_%d" % (t, kc)], writes=["hs%d" % s])
            q = self.rot("sq", 2)
            self._act(self.sq[q][:, :], self.hs[s][:, :], AF.Square, ["hs%d" % s], ["sq%d" % q])
            self.mm(ps[:, :], pres, self.ones[:, :], self.sq[q][:, :], ["const1", "sq%d" % q],
                    kc == 0, kc == KC - 1, inc=True)
        self._ts("dve", self.rt[:, :], ps[:, :], 1.0 / D, EPS, ALU.mult, ALU.add, [pres], ["rt"])
        self._act(self.rt[:, :], self.rt[:, :], AF.Sqrt, ["rt"], ["rt"])
        rstd, rt = self.rstd, self.rt
        P.op("dve", lambda e: e.reciprocal(rstd[:, :], rt[:, :]), reads=["rt"], writes=["rstd"])
        for kc in range(KC):
            s = self.rot("hs", 4)
            P.dma("sp", "hs%d" % s, self.hs[s][:, :], srcv[:, kc, cols], reads=["h%d_%d" % (t, kc)], writes=["hs%d" % s])
            if final_dst is None:
                self._stt("dve", self.hn[:, kc, :], self.hs[s][:, :], vec[:, kc:kc + 1], self.rstd[:, :],
                          ALU.mult, ALU.mult, ["hs%d" % s, "rstd", "vec%d" % li], ["hn%d" % kc])
            else:
                o = self.rot("hnew", 2)
                self._stt("dve", self.hnew[o][:, :], self.hs[s][:, :], vec[:, kc:kc + 1], self.rstd[:, :],
                          ALU.mult, ALU.mult, ["hs%d" % s, "rstd", "vec%d" % li], ["hnew%d" % o])
                dstv = final_dst.rearrange("(k p) t -> p k t", p=128)
                P.dma("sp", "ho%d" % o, dstv[:, kc, cols], self.hnew[o][:, :],
                      reads=["hnew%d" % o], writes=["f%d_%d" % (t, kc)])
                self.out_res.add("f%d_%d" % (t, kc))

    def fnorm_layer(self, li, src, dst):
        for t in range(self.NT):
            self.add_task(None, lambda slot, t=t: self.norm_stage(li, src, t, final_dst=dst))

    def out_tasks(self, li, src, dst, t):
        P = self.P
        w_out = self.lw[li]["w_out"].rearrange("(k p) c -> p k c", p=128)
        cols = slice(t * TT, (t + 1) * TT)
        srcv = src.rearrange("(k p) t -> p k t", p=128)
        dstv = dst.rearrange("(k p) t -> p k t", p=128)
        for j in range(8):
            def load(slot, j=j):
                Wv = self.W[slot][:, :].rearrange("p (k c) -> p k c", k=32)
                self.wload(slot, Wv, w_out[:, :, j * 256:(j + 1) * 256])

            def compute(slot, j=j):
                Wv = self.W[slot][:, :].rearrange("p (k c) -> p k c", k=32)
                for dl in range(2):
                    dc = j * 2 + dl
                    ps, pres = self.bank()
                    for kc in range(32):
                        self.mm(ps[:, :], pres, Wv[:, kc, dl * 128:(dl + 1) * 128], self.gT[:, kc, :],
                                ["W%d" % slot, "gT%d" % kc], kc == 0, kc == 31)
                    s = self.rot("hs", 4)
                    hres = "h%d_%d" % (t, dc)
                    P.dma("sp", "hs%d" % s, self.hs[s][:, :], srcv[:, dc, cols], reads=[hres], writes=["hs%d" % s])
                    o = self.rot("hnew", 2)
                    self._tt("dve", self.hnew[o][:, :], ps[:, :], self.hs[s][:, :], ALU.add,
                             [pres, "hs%d" % s], ["hnew%d" % o])
                    P.dma("sp", "ho%d" % o, dstv[:, dc, cols], self.hnew[o][:, :],
                          reads=["hnew%d" % o], writes=[hres])
                    self.out_res.add(hres)
            self.add_task(load, compute, key=(li, t) if self.NT > 1 else None)

    def pool_layer(self, li, src, dst):
        P = self.P
        p = self.lw[li]
        vec = self.vec[li]
        w_in = p["w_in"].rearrange("(k p) c -> p k c", p=128)
        w_grp = p["w_grp"].rearrange("g (k p) c -> g p k c", p=128)
        uprev = self.uprev
        self.add_task(None, lambda slot: P.op("pool", lambda e: e.memset(uprev[:, :], 0.0), writes=["uprev"]))
        for t in range(self.NT):
            self.add_task(None, lambda slot, t=t: self.norm_stage(li, src, t))
            for j in range(8):
                def load(slot, j=j):
                    Wv = self.W[slot][:, :].rearrange("p (k c) -> p k c", k=16)
                    self.wload(slot, Wv, w_in[:, :, j * 512:(j + 1) * 512])

                def compute(slot, j=j):
                    Wv = self.W[slot][:, :].rearrange("p (k c) -> p k c", k=16)
                    for tc in range(4):
                        ps, pres = self.bank()
                        for kc in range(KC):
                            self.mm(ps[:, :], pres, self.hn[:, kc, tc * 128:(tc + 1) * 128], Wv[:, kc, :],
                                    ["hn%d" % kc, "W%d" % slot], kc == 0, kc == KC - 1)
                        dstu = self.u[:, tc, j * 512:(j + 1) * 512]
                        if (tc + j) % 2 == 0:
                            self._act(dstu, ps[:, :], AF.Copy, [pres], ["u%d_%d" % (tc, j)])
                        else:
                            self._copy("dve", dstu, ps[:, :], [pres], ["u%d_%d" % (tc, j)])
                self.add_task(load, compute, key=(li, t) if self.NT > 1 else None)
            for j in range(8):
                def load(slot, j=j):
                    Wv = self.W[slot][:, :].rearrange("p (k c) -> p k c", k=16)
                    self.wload(slot, Wv, w_in[:, :, 4096 + j * 512:4096 + (j + 1) * 512])

                def compute(slot, j=j):
                    Wv = self.W[slot][:, :].rearrange("p (k c) -> p k c", k=16)
                    for q in range(4):
                        oc = j * 4 + q
                        ps, pres = self.bank()
                        for kc in range(KC):
                            self.mm(ps[:, :], pres, Wv[:, kc, q * 128:(q + 1) * 128], self.hn[:, kc, :],
                                    ["W%d" % slot, "hn%d" % kc], kc == 0, kc == KC - 1)
                        self._act(self.gT[:, oc, :], ps[:, :], AF.Silu, [pres], ["gT%d" % oc])
                self.add_task(load, compute, key=(li, t) if self.NT > 1 else None)
            for g in range(4):
                def load(slot, g=g):
                    Wv = self.W[slot][:, :].rearrange("p (k c) -> p k c", k=8)
                    self.wload(slot, Wv, w_grp[g])

                def compute(slot, g=g, t=t):
                    Wv = self.W[slot][:, :].rearrange("p (k c) -> p k c", k=8)
                    mx = self.mixs[g % 2]
                    mres = "mixs%d" % (g % 2)
                    j0 = g * 2
                    for cl in range(8):
                        cc = g * 8 + cl
                        ublk = cc // 4
                        ps, pres = self.bank()
                        for tc in range(4):
                            first = (t == 0 and tc == 0)
                            A = (self.afirst if first else self.acur)
                            ares = "afirst" if first else "acur"
                            cur = self.u[:, tc, cc * 128:(cc + 1) * 128]
                            if tc == 0:
                                prv = self.uprev[:, cc * 128:(cc + 1) * 128]
                                prv_res = "uprev"
                            else:
                                prv = self.u[:, tc - 1, cc * 128:(cc + 1) * 128]
                                prv_res = "u%d_%d" % (tc - 1, ublk)
                            o = ps[:, tc * 128:(tc + 1) * 128]
                            self.mm(o, pres, cur, A[:, g, :], ["u%d_%d" % (tc, ublk), ares], True, False, inc=False)
                            self.mm(o, pres, prv, self.aprev[:, g, :], [prv_res, "aprev"], False, True,
                                    inc=(tc == 3))
                        if t == 0:
                            self._tt("dve", mx[:, cl, 0:128], ps[:, 0:128], self.invc[:, g, :], ALU.mult,
                                     [pres, "invc"], [mres + "_%d" % cl])
                            self._ts("dve", mx[:, cl, 128:TT], ps[:, 128:TT], 1.0 / WIN[g], None, ALU.mult, None,
                                     [pres], [mres + "_%db" % cl])
                        else:
                            self._ts("dve", mx[:, cl, :], ps[:, :], 1.0 / WIN[g], None, ALU.mult, None,
                                     [pres], [mres + "_%d" % cl, mres + "_%db" % cl])
                    for jd in range(8):
                        oc = g * 8 + jd
                        ps, pres = self.bank()
                        for kc in range(8):
                            self.mm(ps[:, :], pres, Wv[:, kc, jd * 128:(jd + 1) * 128], mx[:, kc, :],
                                    ["W%d" % slot, mres + "_%d" % kc, mres + "_%db" % kc], kc == 0, kc == 7)
                        m = self.rot("m2", 2)
                        self._act(self.m2[m][:, :], ps[:, :], AF.Identity, [pres, "vec%d" % li, "bs%d" % li],
                                  ["m2_%d" % m], bias=self.bs[li][:, oc:oc + 1], scale=vec[:, 48 + oc:49 + oc])
                        eng = "dve"
                        self._tt(eng, self.gT[:, oc, :], self.m2[m][:, :], self.gT[:, oc, :], ALU.mult,
                                 ["m2_%d" % m, "gT%d" % oc], ["gT%d" % oc])
                    if g == 3:
                        ures = ["u3_%d" % b for b in range(8)]
                        self._act(self.uprev[:, :], self.u[:, 3, :], AF.Copy, ures, ["uprev"])
                self.add_task(load, compute, key=(li, t) if self.NT > 1 else None)
            self.out_tasks(li, src, dst, t)

    def ret_layer(self, li, src, dst):
        P = self.P
        p = self.lw[li]
        vec = self.vec[li]
        w_in = p["w_in"].rearrange("(k p) c -> p k c", p=128)
        S = self.S
        self.add_task(None, lambda slot: P.op("pool", lambda e: e.memset(S[:, :, :, :], 0.0),
                                              writes=["S%d" % h for h in range(NH)]))
        for t in range(self.NT):
            def pre(slot, t=t):
                self.norm_stage(li, src, t)
                cols = slice(t * TT, (t + 1) * TT)
                P.dma("sp", "cos", self.cos[:, :], self.cd["cosT"][:, cols], writes=["cos"])
                P.dma("sp", "sin", self.sin[:, :], self.cd["sinT"][:, cols], writes=["sin"])
            self.add_task(None, pre)
            for h in range(NH):
                hb = 0

                def load_qk(slot, h=h):
                    Wv = self.W[slot][:, :].rearrange("p (k c) -> p k c", k=16)
                    self.wload(slot, Wv[:, :, 0:256], w_in[:, :, h * 256:(h + 1) * 256])
                    self.wload(slot, Wv[:, :, 256:512], w_in[:, :, 2048 + h * 256:2048 + (h + 1) * 256])

                def comp_qk(slot, h=h, hb=hb):
                    Wv = self.W[slot][:, :].rearrange("p (k c) -> p k c", k=16)
                    for wi, (dstT, dres) in enumerate(((self.qT[hb], "qT%d" % hb), (self.kT[hb], "kT%d" % hb))):
                        pa, para = self.bank()
                        pb, parb = self.bank()
                        for half, (ps, pres) in enumerate(((pa, para), (pb, parb))):
                            c0 = wi * 256 + half * 128
                            for kc in range(KC):
                                self.mm(ps[:, :], pres, Wv[:, kc, c0:c0 + 128], self.hn[:, kc, :],
                                        ["W%d" % slot, "hn%d" % kc], kc == 0, kc == KC - 1)
                        r = self.rtmp
                        self._tt("dve", r[0][:, :], pa[:, :], self.cos[:, :], ALU.mult, [para, "cos"], ["rtmp0"])
                        self._tt("dve", r[1][:, :], pb[:, :], self.sin[:, :], ALU.mult, [parb, "sin"], ["rtmp1"])
                        self._tt("dve", dstT[:, 0, :], r[0][:, :], r[1][:, :], ALU.subtract,
                                 ["rtmp0", "rtmp1"], [dres])
                        self._tt("dve", r[2][:, :], pa[:, :], self.sin[:, :], ALU.mult, [para, "sin"], ["rtmp2"])
                        self._tt("dve", r[3][:, :], pb[:, :], self.cos[:, :], ALU.mult, [parb, "cos"], ["rtmp3"])
                        self._tt("dve", dstT[:, 1, :], r[2][:, :], r[3][:, :], ALU.add,
                                 ["rtmp2", "rtmp3"], [dres])
                self.add_task(load_qk, comp_qk, key=(li, t) if self.NT > 1 else None)

                def load_v(slot, h=h):
                    Wv = self.W[slot][:, :].rearrange("p (k c) -> p k c", k=16)
                    self.wload(slot, Wv, w_in[:, :, 4096 + h * 512:4096 + (h + 1) * 512])

                def comp_v(slot, h=h, hb=hb, t=t):
                    Wv = self.W[slot][:, :].rearrange("p (k c) -> p k c", k=16)
                    vb, vres = self.v[hb], "v%d" % hb
                    for tc in range(4):
                        ps, pres = self.bank()
                        for kc in range(KC):
                            self.mm(ps[:, :], pres, self.hn[:, kc, tc * 128:(tc + 1) * 128], Wv[:, kc, :],
                                    ["hn%d" % kc, "W%d" % slot], kc == 0, kc == KC - 1)
                        self._act(vb[:, tc, :], ps[:, :], AF.Copy, [pres], [vres + "_%d" % tc])
                    self.ret_core(li, h, hb)
                self.add_task(load_v, comp_v, key=(li, t) if self.NT > 1 else None)

                def load_z(slot, h=h):
                    Wv = self.W[slot][:, :].rearrange("p (k c) -> p k c", k=16)
                    self.wload(slot, Wv, w_in[:, :, 8192 + h * 512:8192 + (h + 1) * 512])

                def comp_z(slot, h=h, hb=hb):
                    Wv = self.W[slot][:, :].rearrange("p (k c) -> p k c", k=16)
                    onb, onres = self.on[hb], "on%d" % hb
                    for ec in range(4):
                        oc = h * 4 + ec
                        ps, pres = self.bank()
                        for kc in range(KC):
                            self.mm(ps[:, :], pres, Wv[:, kc, ec * 128:(ec + 1) * 128], self.hn[:, kc, :],
                                    ["W%d" % slot, "hn%d" % kc], kc == 0, kc == KC - 1)
                        z = self.rot("sz", 2)
                        self._act(self.sz[z][:, :], ps[:, :], AF.Silu, [pres], ["sz%d" % z])
                        pT, ptres = self.bankT()
                        for tc in range(4):
                            self.tr(pT[:, tc * 128:(tc + 1) * 128], ptres, onb[:, tc, ec * 128:(ec + 1) * 128],
                                    [onres + "_%d" % tc], inc=(tc == 3))
                        self._stt("dve", self.gT[:, oc, :], pT[:, 0:TT], vec[:, 16 + oc:17 + oc], self.sz[z][:, :],
                                  ALU.mult, ALU.mult, [ptres, "vec%d" % li, "sz%d" % z], ["gT%d" % oc])
                self.add_task(load_z, comp_z, key=(li, t) if self.NT > 1 else None)
            self.out_tasks(li, src, dst, t)

    def ret_core(self, li, h, hb):
        P = self.P
        qT, kT, vb, onb = self.qT[hb], self.kT[hb], self.v[hb], self.on[hb]
        qres, kres, vres, onres = "qT%d" % hb, "kT%d" % hb, "v%d" % hb, "on%d" % hb
        S = self.S
        Sres = "S%d" % h
        g128 = self.cdec[h]
        for dch in range(2):
            self._act(self.Sbf[0][:, dch, :], S[:, h, dch, :], AF.Copy, [Sres], ["Sbf0_%d" % dch])
        for tc in range(4):
            par = tc % 2
            tcs = slice(tc * 128, (tc + 1) * 128)
            pT, ptres = self.bankT()
            for dch in range(2):
                self.tr(pT[:, dch * 128:(dch + 1) * 128], ptres, kT[:, dch, tcs], [kres], inc=(dch == 1))
            x = self.rot("kd", 2)
            self._act(self.kd[x][:, :], pT[:, 0:256], AF.Identity, [ptres, "kdec"], ["kd%d" % x],
                      scale=self.kdec[:, h:h + 1])
            pss, psres = self.bank()
            for dch in range(2):
                self.mm(pss[:, 0:128], psres, kT[:, dch, tcs], qT[:, dch, tcs], [kres, qres], dch == 0, dch == 1)
            y = self.rot("sm", 2)
            self._tt("dve", self.sm[y][:, :], pss[:, 0:128], self.mask[:, h, :], ALU.mult,
                     [psres, "mask"], ["sm%d" % y])
            psS = []
            for dch in range(2):
                ps, pres = self.bank()
                self.mm(ps[:, :], pres, self.kd[x][:, dch * 128:(dch + 1) * 128], vb[:, tc, :],
                        ["kd%d" % x, vres + "_%d" % tc], True, True)
                psS.append((ps, pres))
            pso, pores = self.bank()
            self.mm(pso[:, :], pores, self.sm[y][:, :], vb[:, tc, :], ["sm%d" % y, vres + "_%d" % tc], True, False,
                    inc=False)
            for dch in range(2):
                self.mm(pso[:, :], pores, qT[:, dch, tcs], self.Sbf[par][:, dch, :],
                        [qres, "Sbf%d_%d" % (par, dch)], False, dch == 1)
            for dch in range(2):
                ps, pres = psS[dch]
                self._stt("dve", S[:, h, dch, :], S[:, h, dch, :], g128, ps[:, :], ALU.mult, ALU.add,
                          [Sres, pres], [Sres])
                if tc < 3:
                    self._act(self.Sbf[1 - par][:, dch, :], S[:, h, dch, :], AF.Copy, [Sres],
                              ["Sbf%d_%d" % (1 - par, dch)])
            orw = self.oraw4
            self._act(orw[:, tc, :], pso[:, :], AF.Identity, [pores, "qdec"], ["oraw_%d" % tc],
                      scale=self.qdec[:, h:h + 1])
            m = self.rot("stat", 4)
            st6, mv4 = self.st6[m], self.mv4
            P.op("dve", lambda e, st6=st6, tc=tc: e.bn_stats(st6[:, :], orw[:, tc, :]),
                 reads=["oraw_%d" % tc], writes=["st6_%d" % m])
            P.op("dve", lambda e, st6=st6, tc=tc: e.bn_aggr(mv4[:, tc, :], st6[:, :]),
                 reads=["st6_%d" % m], writes=["mv4_%d" % tc])
        sd4, rs4, mv4, orw = self.sd4, self.rs4, self.mv4, self.oraw4
        mvr = ["mv4_%d" % tc for tc in range(4)]
        self._act(sd4[:, :], mv4[:, :, 1], AF.Sqrt, mvr + ["epsb"], ["sd4"], bias=self.epsb[:, 0:1], scale=1.0)
        P.op("dve", lambda e: e.reciprocal(rs4[:, :], sd4[:, :]), reads=["sd4"], writes=["rs4"])
        for tc in range(4):
            self._ts("dve", onb[:, tc, :], orw[:, tc, :], mv4[:, tc, 0:1], rs4[:, tc:tc + 1], ALU.subtract, ALU.mult,
                     ["oraw_%d" % tc, "mv4_%d" % tc, "rs4"], [onres + "_%d" % tc])


_CACHE = {}


def get_prog(T, layers):
    key = (T, tuple(layers))
    if key not in _CACHE:
        _CACHE[key] = Builder(T, list(layers)).build()
    return _CACHE[key]


def layer_inputs(li, kind, j, inp):
    d = {}
    if kind == "ret":
        d["l%d_norm" % li] = vec_layout(inp["ret_norm"][j])
        d["l%d_w_in" % li] = np.ascontiguousarray(inp["ret_w_in"][j], dtype=np.float32)
        d["l%d_gn" % li] = vec_layout(inp["ret_gn"][j])
        d["l%d_w_out" % li] = np.ascontiguousarray(inp["ret_w_out"][j], dtype=np.float32)
    elif kind == "pool":
        d["l%d_norm" % li] = vec_layout(inp["pool_norm"][j])
        d["l%d_w_in" % li] = np.ascontiguousarray(inp["pool_w_in"][j], dtype=np.float32)
        d["l%d_w_grp" % li] = np.ascontiguousarray(inp["pool_w_grp"][j], dtype=np.float32)
        d["l%d_b" % li] = vec_layout(np.asarray(inp["pool_b_grp"][j]).reshape(-1))
        d["l%d_s" % li] = vec_layout(inp["pool_scale"][j])
        d["l%d_w_out" % li] = np.ascontiguousarray(inp["pool_w_out"][j], dtype=np.float32)
    else:
        d["l%d_norm" % li] = vec_layout(inp["final_norm"])
    return d


FUSED = True


def kernel(**inputs):
    inp = {k: np.asarray(v) for k, v in inputs.items()}
    x = inp["x"].astype(np.float32, copy=False)
    B, S, _ = x.shape
    consts, _ = const_tables(S)
    hT = [np.ascontiguousarray(x[b].T) for b in range(B)]
    cores = list(range(B))
    plan = [("ret", 0), ("pool", 0), ("ret", 1), ("pool", 1), ("fnorm", 0)]
    if FUSED:
        stages = [plan]
    else:
        stages = [[s] for s in plan]
    for stage in stages:
        kinds = [k for k, _ in stage]
        nc = get_prog(S, kinds)
        shared = {k: v for k, v in consts.items() if k in const_names(kinds)}
        for li, (kind, j) in enumerate(stage):
            shared.update(layer_inputs(li, kind, j, inp))
        in_maps = []
        for b in range(B):
            m = dict(shared)
            m["hin"] = hT[b]
            in_maps.append(m)
        res = run_bass_kernel_spmd(nc, in_maps, core_ids=cores)
        hT = [np.asarray(res.results[b]["hout"]) for b in range(B)]
    out = np.stack([h.T for h in hT], axis=0).astype(np.float32)
    return out
```

```python
import contextlib
import numpy as np
import concourse.bass as bass
import concourse.mybir as mybir
from concourse.bass_utils import run_bass_kernel_spmd

F32 = mybir.dt.float32
BF16 = mybir.dt.bfloat16
AF = mybir.ActivationFunctionType
ALU = mybir.AluOpType

D = 2048
KC = 16
TT = 512
NH = 8
EPS = 1e-6
SEQ = 4096
BATCH = 4
NSLOT = 3
WIN = (2, 4, 8, 16)

ENGS = ("pe", "dve", "act", "pool", "sp")


class Prog:
    def __init__(self, nc):
        self.nc = nc
        self.streams = {e: [] for e in ENGS}
        self.count = {}
        self.known = {e: {} for e in ENGS}
        self.lastw = {}
        self.readers = {}
        self.dma_keys = set()

    def _deps(self, eng, reads, writes):
        deps = {}

        def need(kv, same_ok):
            if kv is None:
                return
            k, v = kv
            if k == eng and same_ok:
                return
            if deps.get(k, 0) < v:
                deps[k] = v

        for r in reads:
            need(self.lastw.get(r), same_ok=(eng == "pe"))
        for w in writes:
            need(self.lastw.get(w), same_ok=True)
            for k, v in self.readers.get(w, {}).items():
                need((k, v), same_ok=True)
        kn = self.known[eng]
        out = []
        for k, v in deps.items():
            if kn.get(k, 0) < v:
                kn[k] = v
                out.append((k, v))
        return out

    def _commit(self, key, val, reads, writes):
        for r in reads:
            self.readers.setdefault(r, {})[key] = val
        for w in writes:
            self.lastw[w] = (key, val)
            self.readers[w] = {}

    def op(self, eng, fn, reads=(), writes=(), inc=True):
        waits = self._deps(eng, reads, writes)
        cur = self.count.get(eng, 0)
        if inc:
            self.count[eng] = cur + 1
        self.streams[eng].append((waits, fn, eng, 1 if inc else 0))
        self._commit(eng, cur + 1, reads, writes)

    def dma(self, q, dsem, out, in_, reads=(), writes=()):
        key = "d:" + dsem
        self.dma_keys.add(key)
        waits = self._deps(q, reads, writes)
        self.count[key] = self.count.get(key, 0) + 16
        self.streams[q].append(
            (waits, lambda e: e.dma_start(out=out, in_=in_), key, 16))
        self._commit(key, self.count[key], reads, writes)

    def dmaop(self, q, dsem, fn, reads=(), writes=()):
        key = "d:" + dsem
        self.dma_keys.add(key)
        waits = self._deps(q, reads, writes)
        self.count[key] = self.count.get(key, 0) + 16
        self.streams[q].append((waits, fn, key, 16))
        self._commit(key, self.count[key], reads, writes)

    def fence(self):
        for eng in ENGS:
            waits = []
            for k, v in self.count.items():
                if k == eng:
                    continue
                if self.known[eng].get(k, 0) < v:
                    self.known[eng][k] = v
                    waits.append((k, v))
            self.streams[eng].append((waits, None, None, 0))

    def wait_all(self, eng, res_list):
        waits = self._deps(eng, res_list, ())
        self.streams[eng].append((waits, None, None, 0))

    def emit(self):
        nc = self.nc
        keys = [e for e in ENGS if e != "sp"] + sorted(self.dma_keys)
        with contextlib.ExitStack() as st:
            sems = {k: st.enter_context(nc.semaphore("s_" + k.replace(":", "_")))
                    for k in keys}
            block = st.enter_context(nc.Block())

            def run(e, stream):
                for waits, fn, key, inc in stream:
                    for k, v in waits:
                        e.wait_ge(sems[k], v)
                    if fn is not None:
                        ins = fn(e)
                        if inc:
                            ins.then_inc(sems[key], inc)

            @block.tensor
            def _(e):
                run(e, self.streams["pe"])

            @block.vector
            def _(e):
                run(e, self.streams["dve"])

            @block.scalar
            def _(e):
                run(e, self.streams["act"])

            @block.gpsimd
            def _(e):
                run(e, self.streams["pool"])

            @block.sync
            def _(e):
                run(e, self.streams["sp"])


def const_tables(T):
    half = 128
    inv = 10000.0 ** (-np.arange(half, dtype=np.float64) / half)
    ang = inv[:, None] * np.arange(T, dtype=np.float64)[None, :]
    cosT = np.cos(ang).astype(np.float32)
    sinT = np.sin(ang).astype(np.float32)
    hh = np.arange(NH, dtype=np.float64)
    lg = np.log1p(-np.exp2(-5.0 - hh))
    idx = np.arange(128, dtype=np.float64)
    tri = (idx[None, :] >= idx[:, None]).astype(np.float64)
    maskT = np.exp(-(idx[:, None, None] + 1.0) * lg[None, :, None]) / 16.0 * tri[:, None, :]
    qdec = np.exp((idx[:, None] + 1.0) * lg[None, :])
    kdec = np.exp((127.0 - idx[:, None]) * lg[None, :]) / 16.0
    cdec = [float(np.exp(128.0 * l)) for l in lg]
    ident = np.eye(128, dtype=np.float32)
    ones = np.ones((128, 128), dtype=np.float32)
    acur = np.zeros((128, 4, 128), np.float32)
    aprev = np.zeros((128, 4, 128), np.float32)
    afirst = np.zeros((128, 4, 128), np.float32)
    invc = np.zeros((128, 4, 128), np.float32)
    for g, w in enumerate(WIN):
        for t in range(128):
            for tp in range(t - w + 1, t + 1):
                if tp >= 0:
                    acur[tp, g, t] += 1.0
                    afirst[tp, g, t] += 1.0
                else:
                    aprev[128 + tp, g, t] += 1.0
            acur[t, g, t] -= float(w)
            cnt = min(t + 1, w)
            afirst[t, g, t] -= float(cnt)
            invc[:, g, t] = 1.0 / cnt
    return dict(cosT=cosT, sinT=sinT, maskT=maskT.astype(np.float32).reshape(128, NH * 128),
                qdec=qdec.astype(np.float32), kdec=kdec.astype(np.float32),
                ident=ident, ones=ones,
                acur=acur.reshape(128, 512), aprev=aprev.reshape(128, 512),
                afirst=afirst.reshape(128, 512), invc=invc.reshape(128, 512)), cdec


CONST_SHAPES = lambda T: dict(cosT=[128, T], sinT=[128, T], maskT=[128, NH * 128], qdec=[128, NH],
                              kdec=[128, NH], ident=[128, 128], ones=[128, 128], acur=[128, 512],
                              aprev=[128, 512], afirst=[128, 512], invc=[128, 512])


def const_names(kinds):
    names = {"ones"}
    if "ret" in kinds:
        names |= {"cosT", "sinT", "maskT", "qdec", "kdec", "ident"}
    if "pool" in kinds:
        names |= {"acur", "aprev", "afirst", "invc"}
    return names


def vec_layout(v):
    v = np.asarray(v, np.float32).reshape(-1, 128)
    return np.ascontiguousarray(v.T)


class Builder:
    def __init__(self, T, layers):
        self.T = T
        self.NT = T // TT
        self.layers = layers
        nc = bass.Bass("TRN2", target_bir_lowering=False)
        self.nc = nc
        self.P = Prog(nc)
        self.st = contextlib.ExitStack()
        self.tasks = []
        self.uoff = {}
        self.blkctr = {}
        self.UBYTES = 78 * 1024
        self.out_res = set()
        self.nwt = 0
        self.ctr = {}
        _, self.cdec = const_tables(128)

    def rot(self, name, n):
        c = self.ctr.get(name, 0)
        self.ctr[name] = c + 1
        return c % n

    def sb(self, name, shape, dt):
        return self.st.enter_context(self.nc.sbuf_tensor("sb_" + name, shape, dt))

    def carve(self, setname, shape, dt):
        nbytes = int(np.prod(shape[1:])) * (4 if dt == F32 else 2)
        nbytes = (nbytes + 63) // 64 * 64
        off = self.uoff.get(setname, 0)
        self.uoff[setname] = off + nbytes
        assert off + nbytes <= self.UBYTES, (setname, off + nbytes)
        v = self.U[:, off // 2:(off + nbytes) // 2]
        if dt == F32:
            v = v.bitcast(F32)
        n = int(np.prod(shape[1:]))
        v = v[:, 0:n]
        if len(shape) == 3:
            v = v.rearrange("p (a b) -> p a b", a=shape[1])
        elif len(shape) == 4:
            v = v.rearrange("p (a b c) -> p a b c", a=shape[1], b=shape[2])
        return v

    def bank(self):
        i = self.rot("bank", 6)
        return self.psA[i], "psA%d" % i

    def bankT(self):
        i = self.rot("bankT", 2)
        return self.psT[i], "psT%d" % i

    def mm(self, out, ores, lhsT, rhs, reads, start, stop, inc=None):
        if inc is None:
            inc = stop
        self.P.op("pe", lambda e: e.matmul(out, lhsT, rhs, start=start, stop=stop),
                  reads=reads, writes=[ores], inc=inc)

    def tr(self, out, ores, in_, reads, inc):
        ident = self.ident
        self.P.op("pe", lambda e: e.transpose(out, in_, ident[:, :]),
                  reads=list(reads) + ["const"], writes=[ores], inc=inc)

    def add_task(self, load, compute, key=None):
        if load is not None:
            slot = self.nwt % NSLOT
            self.nwt += 1
            if key is not None:
                li, t = key
                blk = self.blkctr.get(key, 0)
                self.blkctr[key] = blk + 1
                wsc = self.wsc[li]
                res = "wsc%d_%d" % (li, blk)
                load_fp32 = load
                if t == 0:
                    def load(slot, load_fp32=load_fp32, wsc=wsc, blk=blk, res=res):
                        load_fp32(slot)
                        self.P.dma("sp", "wb%d" % slot, wsc[blk], self.W[slot][:, :],
                                   reads=["W%d" % slot], writes=[res])
                else:
                    def load(slot, wsc=wsc, blk=blk, res=res):
                        self.P.dma("sp", "w%d" % slot, self.W[slot][:, :], wsc[blk],
                                   reads=[res], writes=["W%d" % slot])
        else:
            slot = None
        self.tasks.append((load, compute, slot))

    def wload(self, slot, dst_view, src):
        self.P.dma("pool", "w%d" % slot, dst_view, src, writes=["W%d" % slot])

    def build(self):
        nc, T = self.nc, self.T
        dram_in = lambda name, shape: nc.dram_tensor(name, shape, F32, kind="ExternalInput").ap()
        self.hin = dram_in("hin", [D, T])
        self.hout = nc.dram_tensor("hout", [D, T], F32, kind="ExternalOutput").ap()
        self.hbuf = nc.dram_tensor("hbuf", [D, T], F32).ap() if len(self.layers) > 1 else None
        self.cd = {k: dram_in(k, s) for k, s in CONST_SHAPES(T).items() if k in const_names(self.layers)}
        self.lw = []
        for li, kind in enumerate(self.layers):
            p = {}
            if kind == "ret":
                p["norm"] = dram_in("l%d_norm" % li, [128, 16])
                p["w_in"] = dram_in("l%d_w_in" % li, [D, 12288])
                p["gn"] = dram_in("l%d_gn" % li, [128, 32])
                p["w_out"] = dram_in("l%d_w_out" % li, [4096, D])
            elif kind == "pool":
                p["norm"] = dram_in("l%d_norm" % li, [128, 16])
                p["w_in"] = dram_in("l%d_w_in" % li, [D, 8192])
                p["w_grp"] = dram_in("l%d_w_grp" % li, [4, 1024, 1024])
                p["b"] = dram_in("l%d_b" % li, [128, 32])
                p["s"] = dram_in("l%d_s" % li, [128, 32])
                p["w_out"] = dram_in("l%d_w_out" % li, [4096, D])
            else:
                p["norm"] = dram_in("l%d_norm" % li, [128, 16])
            self.lw.append(p)

        self.wsc = {}
        for li, kind in enumerate(self.layers):
            if kind in ("ret", "pool") and self.NT > 1:
                nblk = 32 if kind == "ret" else 28
                self.wsc[li] = nc.dram_tensor("wsc%d" % li, [nblk, 128, 8192], BF16).ap()
        sb = self.sb
        has_ret = "ret" in self.layers
        has_pool = "pool" in self.layers
        self.W = [sb("W%d" % i, [128, 8192], BF16) for i in range(NSLOT)]
        self.hs = [sb("hs%d" % i, [128, TT], F32) for i in range(4)]
        self.hnew = [sb("hnew%d" % i, [128, TT], F32) for i in range(2)]
        self.hn = sb("hn", [128, KC, TT], BF16)
        self.sq = [sb("sq%d" % i, [128, TT], BF16) for i in range(2)]
        self.rt = sb("rt", [128, TT], F32)
        self.rstd = sb("rstd", [128, TT], F32)
        self.gT = sb("gT", [128, 32, TT], BF16)
        self.ident = sb("ident", [128, 128], BF16)
        self.ones = sb("ones", [128, 128], BF16)
        self.epsb = sb("epsb", [128, 1], F32)
        self.vec = [sb("vec%d" % li, [128, 112], F32) for li in range(len(self.layers))]
        self.U = sb("U", [128, self.UBYTES // 2], BF16)
        if has_ret:
            cv = lambda shape, dt: self.carve("ret", shape, dt)
            self.mask = sb("mask", [128, NH, 128], F32)
            self.qdec = sb("qdec", [128, NH], F32)
            self.kdec = sb("kdec", [128, NH], F32)
            self.cos = cv([128, TT], F32)
            self.sin = cv([128, TT], F32)
            self.S = cv([128, NH, 2, 512], F32)
            self.Sbf = [cv([128, 2, 512], BF16) for i in range(2)]
            self.qT = [cv([128, 2, TT], BF16)]
            self.kT = [cv([128, 2, TT], BF16)]
            self.v = [cv([128, 4, 512], BF16)]
            self.kd = [cv([128, 256], BF16) for i in range(4)]
            self.sm = [cv([128, 128], BF16) for i in range(4)]
            self.oraw4 = cv([128, 4, 512], F32)
            self.mv4 = cv([128, 4, 2], F32)
            self.sd4 = cv([128, 4], F32)
            self.rs4 = cv([128, 4], F32)
            self.on = [cv([128, 4, 512], BF16)]
            self.sz = [cv([128, TT], F32) for i in range(2)]
            self.rtmp = [cv([128, TT], F32) for i in range(4)]
            self.st6 = [cv([128, 6], F32) for i in range(4)]
        if has_pool:
            cv = lambda shape, dt: self.carve("pool", shape, dt)
            self.acur = sb("acur", [128, 4, 128], BF16)
            self.aprev = sb("aprev", [128, 4, 128], BF16)
            self.afirst = sb("afirst", [128, 4, 128], BF16)
            self.invc = sb("invc", [128, 4, 128], F32)
            self.bs = [sb("bs%d" % li, [128, 32], F32) for li in range(len(self.layers))]
            self.u = cv([128, 4, 4096], BF16)
            self.uprev = cv([128, 4096], BF16)
            self.mixs = [cv([128, 8, TT], BF16) for i in range(2)]
            self.m2 = [cv([128, TT], F32) for i in range(2)]
        self.psA = [self.st.enter_context(nc.psum_tensor("psA%d" % i, [128, 512], F32)) for i in range(6)]
        self.psT = [self.st.enter_context(nc.psum_tensor("psT%d" % i, [128, 1024], BF16)) for i in range(2)]

        P = self.P
        if has_ret:
            P.dma("pool", "c0", self.ident[:, :], self.cd["ident"][:, :], writes=["const"])
        P.dma("pool", "c1", self.ones[:, :], self.cd["ones"][:, :], writes=["const1"])
        epsb = self.epsb
        P.op("dve", lambda e: e.memset(epsb[:, :], EPS), writes=["epsb"])
        if has_ret:
            P.dma("sp", "c2", self.mask[:, :, :], self.cd["maskT"].rearrange("p (h i) -> p h i", h=NH), writes=["mask"])
            P.dma("sp", "c3", self.qdec[:, :], self.cd["qdec"][:, :], writes=["qdec"])
            P.dma("sp", "c4", self.kdec[:, :], self.cd["kdec"][:, :], writes=["kdec"])
        if has_pool:
            for nm, key in (("acur", "c5"), ("aprev", "c6"), ("afirst", "c7")):
                P.dma("pool", key, getattr(self, nm)[:, :, :],
                      self.cd[nm].rearrange("p (g t) -> p g t", g=4), writes=[nm])
            P.dma("sp", "c8", self.invc[:, :, :], self.cd["invc"].rearrange("p (g t) -> p g t", g=4), writes=["invc"])
        for li, kind in enumerate(self.layers):
            p = self.lw[li]
            vec = self.vec[li]
            P.dma("sp", "v%d" % li, vec[:, 0:16], p["norm"][:, :], writes=["vec%d" % li])
            if kind == "ret":
                P.dma("sp", "v%d" % li, vec[:, 16:48], p["gn"][:, :], writes=["vec%d" % li])
            elif kind == "pool":
                P.dma("sp", "v%d" % li, vec[:, 16:48], p["b"][:, :], writes=["vec%d" % li])
                P.dma("sp", "v%d" % li, vec[:, 48:80], p["s"][:, :], writes=["vec%d" % li])
                bs = self.bs[li]
                self._tt("dve", bs[:, :], vec[:, 16:48], vec[:, 48:80], ALU.mult,
                         ["vec%d" % li], ["bs%d" % li])

        nl = len(self.layers)
        for li, kind in enumerate(self.layers):
            src = self.hin if li == 0 else self.hbuf
            dst = self.hout if li == nl - 1 else self.hbuf
            if li > 0:
                self.add_task(None, lambda slot: self.P.fence())
            if kind == "ret":
                self.ret_layer(li, src, dst)
            elif kind == "pool":
                self.pool_layer(li, src, dst)
            else:
                self.fnorm_layer(li, src, dst)

        wt = [i for i, t in enumerate(self.tasks) if t[0] is not None]
        nl_issued = 0
        wt_done = 0
        for i, (load, compute, slot) in enumerate(self.tasks):
            while nl_issued < len(wt) and nl_issued < wt_done + NSLOT:
                j = wt[nl_issued]
                self.tasks[j][0](self.tasks[j][2])
                nl_issued += 1
            compute(slot)
            if load is not None:
                wt_done += 1
        P.wait_all("sp", sorted(self.out_res))
        P.emit()
        self.st.close()
        return nc

    def _tt(self, eng, out, in0, in1, op, reads, writes):
        self.P.op(eng, lambda e: e.tensor_tensor(out, in0, in1, op), reads=reads, writes=writes)

    def _ts(self, eng, out, in0, s1, s2, op0, op1, reads, writes):
        if s2 is None:
            self.P.op(eng, lambda e: e.tensor_scalar(out, in0, s1, None, op0), reads=reads, writes=writes)
        else:
            self.P.op(eng, lambda e: e.tensor_scalar(out, in0, s1, s2, op0, op1), reads=reads, writes=writes)

    def _stt(self, eng, out, in0, scalar, in1, op0, op1, reads, writes):
        self.P.op(eng, lambda e: e.scalar_tensor_tensor(out, in0, scalar, in1, op0, op1),
                  reads=reads, writes=writes)

    def _act(self, out, in_, func, reads, writes, bias=None, scale=None):
        kw = {}
        if bias is not None:
            kw["bias"] = bias
        if scale is not None:
            kw["scale"] = scale
        self.P.op("act", lambda e: e.activation(out, in_, func, **kw), reads=reads, writes=writes)

    def _copy(self, eng, out, in_, reads, writes):
        self.P.op(eng, lambda e: e.tensor_copy(out, in_), reads=reads, writes=writes)

    def norm_stage(self, li, src, t, final_dst=None):
        P = self.P
        vec = self.vec[li]
        cols = slice(t * TT, (t + 1) * TT)
        srcv = src.rearrange("(k p) t -> p k t", p=128)
        ps, pres = self.bank()
        for kc in range(KC):
            s = self.rot("hs", 4)
            P.dma("sp", "hs%d" % s, self.hs[s][:, :], srcv[:, kc, cols], reads=["h%d_%d" % (t, kc)], writes=["hs%d" % s])
            q = self.rot("sq", 2)
            self._act(self.sq[q][:, :], self.hs[s][:, :], AF.Square, ["hs%d" % s], ["sq%d" % q])
            self.mm(ps[:, :], pres, self.ones[:, :], self.sq[q][:, :], ["const1", "sq%d" % q],
                    kc == 0, kc == KC - 1, inc=True)
        self._ts("dve", self.rt[:, :], ps[:, :], 1.0 / D, EPS, ALU.mult, ALU.add, [pres], ["rt"])
        self._act(self.rt[:, :], self.rt[:, :], AF.Sqrt, ["rt"], ["rt"])
        rstd, rt = self.rstd, self.rt
        P.op("dve", lambda e: e.reciprocal(rstd[:, :], rt[:, :]), reads=["rt"], writes=["rstd"])
        for kc in range(KC):
            s = self.rot("hs", 4)
            P.dma("sp", "hs%d" % s, self.hs[s][:, :], srcv[:, kc, cols], reads=["h%d_%d" % (t, kc)], writes=["hs%d" % s])
            if final_dst is None:
                self._stt("dve", self.hn[:, kc, :], self.hs[s][:, :], vec[:, kc:kc + 1], self.rstd[:, :],
                          ALU.mult, ALU.mult, ["hs%d" % s, "rstd", "vec%d" % li], ["hn%d" % kc])
            else:
                o = self.rot("hnew", 2)
                self._stt("dve", self.hnew[o][:, :], self.hs[s][:, :], vec[:, kc:kc + 1], self.rstd[:, :],
                          ALU.mult, ALU.mult, ["hs%d" % s, "rstd", "vec%d" % li], ["hnew%d" % o])
                dstv = final_dst.rearrange("(k p) t -> p k t", p=128)
                P.dma("sp", "ho%d" % o, dstv[:, kc, cols], self.hnew[o][:, :],
                      reads=["hnew%d" % o], writes=["f%d_%d" % (t, kc)])
                self.out_res.add("f%d_%d" % (t, kc))

    def fnorm_layer(self, li, src, dst):
        for t in range(self.NT):
            self.add_task(None, lambda slot, t=t: self.norm_stage(li, src, t, final_dst=dst))

    def out_tasks(self, li, src, dst, t):
        P = self.P
        w_out = self.lw[li]["w_out"].rearrange("(k p) c -> p k c", p=128)
        cols = slice(t * TT, (t + 1) * TT)
        srcv = src.rearrange("(k p) t -> p k t", p=128)
        dstv = dst.rearrange("(k p) t -> p k t", p=128)
        for j in range(8):
            def load(slot, j=j):
                Wv = self.W[slot][:, :].rearrange("p (k c) -> p k c", k=32)
                self.wload(slot, Wv, w_out[:, :, j * 256:(j + 1) * 256])

            def compute(slot, j=j):
                Wv = self.W[slot][:, :].rearrange("p (k c) -> p k c", k=32)
                for dl in range(2):
                    dc = j * 2 + dl
                    ps, pres = self.bank()
                    for kc in range(32):
                        self.mm(ps[:, :], pres, Wv[:, kc, dl * 128:(dl + 1) * 128], self.gT[:, kc, :],
                                ["W%d" % slot, "gT%d" % kc], kc == 0, kc == 31)
                    s = self.rot("hs", 4)
                    hres = "h%d_%d" % (t, dc)
                    P.dma("sp", "hs%d" % s, self.hs[s][:, :], srcv[:, dc, cols], reads=[hres], writes=["hs%d" % s])
                    o = self.rot("hnew", 2)
                    self._tt("dve", self.hnew[o][:, :], ps[:, :], self.hs[s][:, :], ALU.add,
                             [pres, "hs%d" % s], ["hnew%d" % o])
                    P.dma("sp", "ho%d" % o, dstv[:, dc, cols], self.hnew[o][:, :],
                          reads=["hnew%d" % o], writes=[hres])
                    self.out_res.add(hres)
            self.add_task(load, compute, key=(li, t) if self.NT > 1 else None)

    def pool_layer(self, li, src, dst):
        P = self.P
        p = self.lw[li]
        vec = self.vec[li]
        w_in = p["w_in"].rearrange("(k p) c -> p k c", p=128)
        w_grp = p["w_grp"].rearrange("g (k p) c -> g p k c", p=128)
        uprev = self.uprev
        self.add_task(None, lambda slot: P.op("pool", lambda e: e.memset(uprev[:, :], 0.0), writes=["uprev"]))
        for t in range(self.NT):
            self.add_task(None, lambda slot, t=t: self.norm_stage(li, src, t))
            for j in range(8):
                def load(slot, j=j):
                    Wv = self.W[slot][:, :].rearrange("p (k c) -> p k c", k=16)
                    self.wload(slot, Wv, w_in[:, :, j * 512:(j + 1) * 512])

                def compute(slot, j=j):
                    Wv = self.W[slot][:, :].rearrange("p (k c) -> p k c", k=16)
                    for tc in range(4):
                        ps, pres = self.bank()
                        for kc in range(KC):
                            self.mm(ps[:, :], pres, self.hn[:, kc, tc * 128:(tc + 1) * 128], Wv[:, kc, :],
                                    ["hn%d" % kc, "W%d" % slot], kc == 0, kc == KC - 1)
                        dstu = self.u[:, tc, j * 512:(j + 1) * 512]
                        if (tc + j) % 2 == 0:
                            self._act(dstu, ps[:, :], AF.Copy, [pres], ["u%d_%d" % (tc, j)])
                        else:
                            self._copy("dve", dstu, ps[:, :], [pres], ["u%d_%d" % (tc, j)])
                self.add_task(load, compute, key=(li, t) if self.NT > 1 else None)
            for j in range(8):
                def load(slot, j=j):
                    Wv = self.W[slot][:, :].rearrange("p (k c) -> p k c", k=16)
                    self.wload(slot, Wv, w_in[:, :, 4096 + j * 512:4096 + (j + 1) * 512])

                def compute(slot, j=j):
                    Wv = self.W[slot][:, :].rearrange("p (k c) -> p k c", k=16)
                    for q in range(4):
                        oc = j * 4 + q
                        ps, pres = self.bank()
                        for kc in range(KC):
                            self.mm(ps[:, :], pres, Wv[:, kc, q * 128:(q + 1) * 128], self.hn[:, kc, :],
                                    ["W%d" % slot, "hn%d" % kc], kc == 0, kc == KC - 1)
                        self._act(self.gT[:, oc, :], ps[:, :], AF.Silu, [pres], ["gT%d" % oc])
                self.add_task(load, compute, key=(li, t) if self.NT > 1 else None)
            for g in range(4):
                def load(slot, g=g):
                    Wv = self.W[slot][:, :].rearrange("p (k c) -> p k c", k=8)
                    self.wload(slot, Wv, w_grp[g])

                def compute(slot, g=g, t=t):
                    Wv = self.W[slot][:, :].rearrange("p (k c) -> p k c", k=8)
                    mx = self.mixs[g % 2]
                    mres = "mixs%d" % (g % 2)
                    j0 = g * 2
                    for cl in range(8):
                        cc = g * 8 + cl
                        ublk = cc // 4
                        ps, pres = self.bank()
                        for tc in range(4):
                            first = (t == 0 and tc == 0)
                            A = (self.afirst if first else self.acur)
                            ares = "afirst" if first else "acur"
                            cur = self.u[:, tc, cc * 128:(cc + 1) * 128]
                            if tc == 0:
                                prv = self.uprev[:, cc * 128:(cc + 1) * 128]
                                prv_res = "uprev"
                            else:
                                prv = self.u[:, tc - 1, cc * 128:(cc + 1) * 128]
                                prv_res = "u%d_%d" % (tc - 1, ublk)
                            o = ps[:, tc * 128:(tc + 1) * 128]
                            self.mm(o, pres, cur, A[:, g, :], ["u%d_%d" % (tc, ublk), ares], True, False, inc=False)
                            self.mm(o, pres, prv, self.aprev[:, g, :], [prv_res, "aprev"], False, True,
                                    inc=(tc == 3))
                        if t == 0:
                            self._tt("dve", mx[:, cl, 0:128], ps[:, 0:128], self.invc[:, g, :], ALU.mult,
                                     [pres, "invc"], [mres + "_%d" % cl])
                            self._ts("dve", mx[:, cl, 128:TT], ps[:, 128:TT], 1.0 / WIN[g], None, ALU.mult, None,
                                     [pres], [mres + "_%db" % cl])
                        else:
                            self._ts("dve", mx[:, cl, :], ps[:, :], 1.0 / WIN[g], None, ALU.mult, None,
                                     [pres], [mres + "_%d" % cl, mres + "_%db" % cl])
                    for jd in range(8):
                        oc = g * 8 + jd
                        ps, pres = self.bank()
                        for kc in range(8):
                            self.mm(ps[:, :], pres, Wv[:, kc, jd * 128:(jd + 1) * 128], mx[:, kc, :],
                                    ["W%d" % slot, mres + "_%d" % kc, mres + "_%db" % kc], kc == 0, kc == 7)
                        m = self.rot("m2", 2)
                        self._act(self.m2[m][:, :], ps[:, :], AF.Identity, [pres, "vec%d" % li, "bs%d" % li],
                                  ["m2_%d" % m], bias=self.bs[li][:, oc:oc + 1], scale=vec[:, 48 + oc:49 + oc])
                        eng = "dve"
                        self._tt(eng, self.gT[:, oc, :], self.m2[m][:, :], self.gT[:, oc, :], ALU.mult,
                                 ["m2_%d" % m, "gT%d" % oc], ["gT%d" % oc])
                    if g == 3:
                        ures = ["u3_%d" % b for b in range(8)]
                        self._act(self.uprev[:, :], self.u[:, 3, :], AF.Copy, ures, ["uprev"])
                self.add_task(load, compute, key=(li, t) if self.NT > 1 else None)
            self.out_tasks(li, src, dst, t)

    def ret_layer(self, li, src, dst):
        P = self.P
        p = self.lw[li]
        vec = self.vec[li]
        w_in = p["w_in"].rearrange("(k p) c -> p k c", p=128)
        S = self.S
        self.add_task(None, lambda slot: P.op("pool", lambda e: e.memset(S[:, :, :, :], 0.0),
                                              writes=["S%d" % h for h in range(NH)]))
        for t in range(self.NT):
            def pre(slot, t=t):
                self.norm_stage(li, src, t)
                cols = slice(t * TT, (t + 1) * TT)
                P.dma("sp", "cos", self.cos[:, :], self.cd["cosT"][:, cols], writes=["cos"])
                P.dma("sp", "sin", self.sin[:, :], self.cd["sinT"][:, cols], writes=["sin"])
            self.add_task(None, pre)
            for h in range(NH):
                hb = 0

                def load_qk(slot, h=h):
                    Wv = self.W[slot][:, :].rearrange("p (k c) -> p k c", k=16)
                    self.wload(slot, Wv[:, :, 0:256], w_in[:, :, h * 256:(h + 1) * 256])
                    self.wload(slot, Wv[:, :, 256:512], w_in[:, :, 2048 + h * 256:2048 + (h + 1) * 256])

                def comp_qk(slot, h=h, hb=hb):
                    Wv = self.W[slot][:, :].rearrange("p (k c) -> p k c", k=16)
                    for wi, (dstT, dres) in enumerate(((self.qT[hb], "qT%d" % hb), (self.kT[hb], "kT%d" % hb))):
                        pa, para = self.bank()
                        pb, parb = self.bank()
                        for half, (ps, pres) in enumerate(((pa, para), (pb, parb))):
                            c0 = wi * 256 + half * 128
                            for kc in range(KC):
                                self.mm(ps[:, :], pres, Wv[:, kc, c0:c0 + 128], self.hn[:, kc, :],
                                        ["W%d" % slot, "hn%d" % kc], kc == 0, kc == KC - 1)
                        r = self.rtmp
                        self._tt("dve", r[0][:, :], pa[:, :], self.cos[:, :], ALU.mult, [para, "cos"], ["rtmp0"])
                        self._tt("dve", r[1][:, :], pb[:, :], self.sin[:, :], ALU.mult, [parb, "sin"], ["rtmp1"])
                        self._tt("dve", dstT[:, 0, :], r[0][:, :], r[1][:, :], ALU.subtract,
                                 ["rtmp0", "rtmp1"], [dres])
                        self._tt("dve", r[2][:, :], pa[:, :], self.sin[:, :], ALU.mult, [para, "sin"], ["rtmp2"])
                        self._tt("dve", r[3][:, :], pb[:, :], self.cos[:, :], ALU.mult, [parb, "cos"], ["rtmp3"])
                        self._tt("dve", dstT[:, 1, :], r[2][:, :], r[3][:, :], ALU.add,
                                 ["rtmp2", "rtmp3"], [dres])
                self.add_task(load_qk, comp_qk, key=(li, t) if self.NT > 1 else None)

                def load_v(slot, h=h):
                    Wv = self.W[slot][:, :].rearrange("p (k c) -> p k c", k=16)
                    self.wload(slot, Wv, w_in[:, :, 4096 + h * 512:4096 + (h + 1) * 512])

                def comp_v(slot, h=h, hb=hb, t=t):
                    Wv = self.W[slot][:, :].rearrange("p (k c) -> p k c", k=16)
                    vb, vres = self.v[hb], "v%d" % hb
                    for tc in range(4):
                        ps, pres = self.bank()
                        for kc in range(KC):
                            self.mm(ps[:, :], pres, self.hn[:, kc, tc * 128:(tc + 1) * 128], Wv[:, kc, :],
                                    ["hn%d" % kc, "W%d" % slot], kc == 0, kc == KC - 1)
                        self._act(vb[:, tc, :], ps[:, :], AF.Copy, [pres], [vres + "_%d" % tc])
                    self.ret_core(li, h, hb)
                self.add_task(load_v, comp_v, key=(li, t) if self.NT > 1 else None)

                def load_z(slot, h=h):
                    Wv = self.W[slot][:, :].rearrange("p (k c) -> p k c", k=16)
                    self.wload(slot, Wv, w_in[:, :, 8192 + h * 512:8192 + (h + 1) * 512])

                def comp_z(slot, h=h, hb=hb):
                    Wv = self.W[slot][:, :].rearrange("p (k c) -> p k c", k=16)
                    onb, onres = self.on[hb], "on%d" % hb
                    for ec in range(4):
                        oc = h * 4 + ec
                        ps, pres = self.bank()
                        for kc in range(KC):
                            self.mm(ps[:, :], pres, Wv[:, kc, ec * 128:(ec + 1) * 128], self.hn[:, kc, :],
                                    ["W%d" % slot, "hn%d" % kc], kc == 0, kc == KC - 1)
                        z = self.rot("sz", 2)
                        self._act(self.sz[z][:, :], ps[:, :], AF.Silu, [pres], ["sz%d" % z])
                        pT, ptres = self.bankT()
                        for tc in range(4):
                            self.tr(pT[:, tc * 128:(tc + 1) * 128], ptres, onb[:, tc, ec * 128:(ec + 1) * 128],
                                    [onres + "_%d" % tc], inc=(tc == 3))
                        self._stt("dve", self.gT[:, oc, :], pT[:, 0:TT], vec[:, 16 + oc:17 + oc], self.sz[z][:, :],
                                  ALU.mult, ALU.mult, [ptres, "vec%d" % li, "sz%d" % z], ["gT%d" % oc])
                self.add_task(load_z, comp_z, key=(li, t) if self.NT > 1 else None)
            self.out_tasks(li, src, dst, t)

    def ret_core(self, li, h, hb):
        P = self.P
        qT, kT, vb, onb = self.qT[hb], self.kT[hb], self.v[hb], self.on[hb]
        qres, kres, vres, onres = "qT%d" % hb, "kT%d" % hb, "v%d" % hb, "on%d" % hb
        S = self.S
        Sres = "S%d" % h
        g128 = self.cdec[h]
        for dch in range(2):
            self._act(self.Sbf[0][:, dch, :], S[:, h, dch, :], AF.Copy, [Sres], ["Sbf0_%d" % dch])
        for tc in range(4):
            tcs = slice(tc * 128, (tc + 1) * 128)
            pT, ptres = self.bankT()
            for dch in range(2):
                self.tr(pT[:, dch * 128:(dch + 1) * 128], ptres, kT[:, dch, tcs], [kres], inc=(dch == 1))
            self._act(self.kd[tc][:, :], pT[:, 0:256], AF.Identity, [ptres, "kdec"], ["kd%d" % tc],
                      scale=self.kdec[:, h:h + 1])
            pss, psres = self.psA[4 + tc % 2], "psA%d" % (4 + tc % 2)
            for dch in range(2):
                self.mm(pss[:, 0:128], psres, kT[:, dch, tcs], qT[:, dch, tcs], [kres, qres], dch == 0, dch == 1)
            self._tt("dve", self.sm[tc][:, :], pss[:, 0:128], self.mask[:, h, :], ALU.mult,
                     [psres, "mask"], ["sm%d" % tc])

        def b_mm(tc):
            for dch in range(2):
                bi = (tc % 2) * 2 + dch
                self.mm(self.psA[bi][:, :], "psA%d" % bi, self.kd[tc][:, dch * 128:(dch + 1) * 128], vb[:, tc, :],
                        ["kd%d" % tc, vres + "_%d" % tc], True, True)

        b_mm(0)
        b_mm(1)
        orw = self.oraw4
        for tc in range(4):
            par = tc % 2
            tcs = slice(tc * 128, (tc + 1) * 128)
            pso, pores = self.psA[4 + par], "psA%d" % (4 + par)
            self.mm(pso[:, :], pores, self.sm[tc][:, :], vb[:, tc, :], ["sm%d" % tc, vres + "_%d" % tc], True, False,
                    inc=False)
            for dch in range(2):
                self.mm(pso[:, :], pores, qT[:, dch, tcs], self.Sbf[par][:, dch, :],
                        [qres, "Sbf%d_%d" % (par, dch)], False, dch == 1)
            for dch in range(2):
                bi = par * 2 + dch
                self._stt("dve", S[:, h, dch, :], S[:, h, dch, :], g128, self.psA[bi][:, :], ALU.mult, ALU.add,
                          [Sres, "psA%d" % bi], [Sres])
                if tc < 3:
                    self._act(self.Sbf[1 - par][:, dch, :], S[:, h, dch, :], AF.Copy, [Sres],
                              ["Sbf%d_%d" % (1 - par, dch)])
            self._act(orw[:, tc, :], pso[:, :], AF.Identity, [pores, "qdec"], ["oraw_%d" % tc],
                      scale=self.qdec[:, h:h + 1])
            m = self.rot("stat", 4)
            st6, mv4 = self.st6[m], self.mv4
            P.op("dve", lambda e, st6=st6, tc=tc: e.bn_stats(st6[:, :], orw[:, tc, :]),
                 reads=["oraw_%d" % tc], writes=["st6_%d" % m])
            P.op("dve", lambda e, st6=st6, tc=tc: e.bn_aggr(mv4[:, tc, :], st6[:, :]),
                 reads=["st6_%d" % m], writes=["mv4_%d" % tc])
            if tc + 2 < 4:
                b_mm(tc + 2)
        sd4, rs4, mv4, orw = self.sd4, self.rs4, self.mv4, self.oraw4
        mvr = ["mv4_%d" % tc for tc in range(4)]
        self._act(sd4[:, :], mv4[:, :, 1], AF.Sqrt, mvr + ["epsb"], ["sd4"], bias=self.epsb[:, 0:1], scale=1.0)
        P.op("dve", lambda e: e.reciprocal(rs4[:, :], sd4[:, :]), reads=["sd4"], writes=["rs4"])
        for tc in range(4):
            self._ts("dve", onb[:, tc, :], orw[:, tc, :], mv4[:, tc, 0:1], rs4[:, tc:tc + 1], ALU.subtract, ALU.mult,
                     ["oraw_%d" % tc, "mv4_%d" % tc, "rs4"], [onres + "_%d" % tc])


_CACHE = {}


def get_prog(T, layers):
    key = (T, tuple(layers))
    if key not in _CACHE:
        _CACHE[key] = Builder(T, list(layers)).build()
    return _CACHE[key]


def layer_inputs(li, kind, j, inp):
    d = {}
    if kind == "ret":
        d["l%d_norm" % li] = vec_layout(inp["ret_norm"][j])
        d["l%d_w_in" % li] = np.ascontiguousarray(inp["ret_w_in"][j], dtype=np.float32)
        d["l%d_gn" % li] = vec_layout(inp["ret_gn"][j])
        d["l%d_w_out" % li] = np.ascontiguousarray(inp["ret_w_out"][j], dtype=np.float32)
    elif kind == "pool":
        d["l%d_norm" % li] = vec_layout(inp["pool_norm"][j])
        d["l%d_w_in" % li] = np.ascontiguousarray(inp["pool_w_in"][j], dtype=np.float32)
        d["l%d_w_grp" % li] = np.ascontiguousarray(inp["pool_w_grp"][j], dtype=np.float32)
        d["l%d_b" % li] = vec_layout(np.asarray(inp["pool_b_grp"][j]).reshape(-1))
        d["l%d_s" % li] = vec_layout(inp["pool_scale"][j])
        d["l%d_w_out" % li] = np.ascontiguousarray(inp["pool_w_out"][j], dtype=np.float32)
    else:
        d["l%d_norm" % li] = vec_layout(inp["final_norm"])
    return d


FUSED = True


def kernel(**inputs):
    inp = {k: np.asarray(v) for k, v in inputs.items()}
    x = inp["x"].astype(np.float32, copy=False)
    B, S, _ = x.shape
    consts, _ = const_tables(S)
    hT = [np.ascontiguousarray(x[b].T) for b in range(B)]
    cores = list(range(B))
    plan = [("ret", 0), ("pool", 0), ("ret", 1), ("pool", 1), ("fnorm", 0)]
    if FUSED:
        stages = [plan]
    else:
        stages = [[s] for s in plan]
    for stage in stages:
        kinds = [k for k, _ in stage]
        nc = get_prog(S, kinds)
        shared = {k: v for k, v in consts.items() if k in const_names(kinds)}
        for li, (kind, j) in enumerate(stage):
            shared.update(layer_inputs(li, kind, j, inp))
        in_maps = []
        for b in range(B):
            m = dict(shared)
            m["hin"] = hT[b]
            in_maps.append(m)
        res = run_bass_kernel_spmd(nc, in_maps, core_ids=cores)
        hT = [np.asarray(res.results[b]["hout"]) for b in range(B)]
    out = np.stack([h.T for h in hT], axis=0).astype(np.float32)
    return out
```
